# Optimizing a Trainium2 kernel written in Bass

```python
import jax
import jax.numpy as jnp
from jax import lax
import numpy as np

D_MODEL = 2048
BATCH = 2
SEQ = 8192
DEPTH = 4

GRID_W = 64
CTX_LEN = 256
MIX_W = D_MODEL
FNET_W = MIX_W // 2
FNET_GROUPS = 4
FNET_GROUP_W = FNET_W // FNET_GROUPS
HG_W = MIX_W // 2
HG_DK = 128
HG_DV = 128
HG_HEADS = HG_W // HG_DV
HG_CHUNK = 64
F_MIN = 1e-30
CONV_W = MIX_W // 2
CONV_K = 31
POOL_W = MIX_W // 2
POOL_WINDOWS = (2, 4, 8, 16)
POOL_GROUPS = len(POOL_WINDOWS)
POOL_GROUP_W = POOL_W // POOL_GROUPS
PEER_KEYS = 128
PEER_EXPERTS = PEER_KEYS * PEER_KEYS
PEER_HEADS = 8
PEER_TOPK = 16
PEER_DKEY = 256
PEER_BLOCK = 128

N_EVEN = (DEPTH + 1) // 2
N_ODD = DEPTH // 2
EVEN_IN = FNET_W + 5 * HG_W
ODD_IN = 2 * CONV_W + POOL_W
N_MOD = 6
EPS = 1e-6

kernel_name = 'hybrid_fnet_hgrn2_conformer_pool_peer_dit'


def rms_norm(x, w):
    xf = x.astype(jnp.float32)
    y = xf * lax.rsqrt(jnp.mean(xf * xf, axis=-1, keepdims=True) + EPS)
    return (y * w.astype(jnp.float32)).astype(x.dtype)


def modulate(h, shift, scale):
    return h * (1 + scale) + shift


def sincos_2d(rows, dim):
    quarter = dim // 4
    row = jnp.repeat(jnp.arange(rows), GRID_W).astype(jnp.float32)[:, None]
    col = jnp.tile(jnp.arange(GRID_W), rows).astype(jnp.float32)[:, None]
    omega = 1.0 / (10000.0 ** (jnp.arange(quarter, dtype=jnp.float32) / quarter))
    ar, ac = row * omega, col * omega
    return jnp.concatenate([jnp.sin(ar), jnp.cos(ar), jnp.sin(ac), jnp.cos(ac)], axis=-1)


def rev_segments(a, n_ctx):
    return jnp.concatenate([jnp.flip(a[:, :n_ctx], 1), jnp.flip(a[:, n_ctx:], 1)], axis=1)


def fourier_mix(a):
    B, L, _ = a.shape
    ag = a.astype(jnp.float32).reshape(B, L, FNET_GROUPS, FNET_GROUP_W)
    out = jnp.fft.fft2(ag, axes=(1, 3), norm='ortho').real
    return out.reshape(B, L, FNET_W).astype(a.dtype)


def hgrn2_chunk_scan(q, k, v, log_f, s0):
    N, L, H, _ = q.shape
    nc = L // HG_CHUNK
    to_chunks = lambda t: t.reshape(N, nc, HG_CHUNK, H, t.shape[-1]).transpose(1, 0, 3, 2, 4)
    mask = jnp.tril(jnp.ones((HG_CHUNK, HG_CHUNK), dtype=bool))[:, :, None]

    def step(S, inp):
        qb, kb, vb, gb = inp
        b = jnp.cumsum(gb, axis=2)
        diff = b[:, :, :, None, :] - b[:, :, None, :, :]
        decay = jnp.where(mask, jnp.exp(jnp.where(mask, diff, 0.0)), 0.0)
        A = jnp.einsum('nhtd,nhsd,nhtsd->nhts', qb, kb, decay)
        o = jnp.einsum('nhts,nhsv->nhtv', A, vb) + jnp.einsum('nhtd,nhdv->nhtv', qb * jnp.exp(b), S)
        b_last = b[:, :, -1:, :]
        S_new = jnp.exp(b_last[:, :, 0, :])[..., None] * S + jnp.einsum(
            'nhsd,nhsv->nhdv', kb * jnp.exp(b_last - b), vb)
        return S_new, o

    _, oc = lax.scan(step, s0, (to_chunks(q), to_chunks(k), to_chunks(v), to_chunks(log_f)))
    return oc.transpose(1, 0, 3, 2, 4).reshape(N, L, H, v.shape[-1])


def fourier_hgrn_mixer(hc, hl, w_in, w_out, lb, norm_w):
    B, n_ctx, _ = hc.shape
    h = jnp.concatenate([hc, hl], axis=1)
    L = h.shape[1]
    z = h @ w_in
    a = z[..., :FNET_W]
    q, fl_fwd, fl_bwd, v, g = jnp.split(z[..., FNET_W:], 5, axis=-1)
    four = jnp.concatenate([fourier_mix(a[:, :n_ctx]), fourier_mix(a[:, n_ctx:])], axis=1)
    heads = lambda t: t.astype(jnp.float32).reshape(B, L, HG_HEADS, -1)
    lb_h = lb.reshape(HG_HEADS, HG_DK)

    def gates(fl):
        sig = jax.nn.sigmoid(heads(fl))
        f = lb_h + (1.0 - lb_h) * sig
        log_f = jnp.log(jnp.maximum(f, F_MIN))
        k = (1.0 - lb_h) * (1.0 - sig)
        return log_f, k

    lf_f, k_f = gates(fl_fwd)
    lf_b, k_b = gates(fl_bwd)
    qh = jax.nn.silu(heads(q))
    vh = heads(v)
    both = lambda tf, tb: jnp.concatenate([tf, rev_segments(tb, n_ctx)], axis=0)
    s0 = jnp.zeros((2 * B, HG_HEADS, HG_DK, HG_DV), jnp.float32)
    o2 = hgrn2_chunk_scan(both(qh, qh), both(k_f, k_b), both(vh, vh), both(lf_f, lf_b), s0)
    o = o2[:B] + rev_segments(o2[B:], n_ctx)
    o = o * lax.rsqrt(jnp.mean(o * o, axis=-1, keepdims=True) + EPS) * norm_w.astype(jnp.float32)
    o = (o.reshape(B, L, HG_W) * jax.nn.silu(g.astype(jnp.float32))).astype(h.dtype)
    y = jnp.concatenate([four, o], axis=-1) @ w_out
    return y[:, :n_ctx], y[:, n_ctx:]


def depthwise_conv(u, w):
    return lax.conv_general_dilated(
        u, w.astype(u.dtype)[:, None, :], window_strides=(1,),
        padding=[(CONV_K // 2, CONV_K // 2)],
        dimension_numbers=('NWC', 'WIO', 'NWC'), feature_group_count=u.shape[-1])


def multiscale_pool(p):
    B, L, _ = p.shape
    pg = p.astype(jnp.float32).reshape(B, L, POOL_GROUPS, POOL_GROUP_W)
    csum = jnp.concatenate([jnp.zeros((B, 1, POOL_GROUPS, POOL_GROUP_W), jnp.float32),
                            jnp.cumsum(pg, axis=1)], axis=1)
    t = jnp.arange(L)
    outs = []
    for gi, w in enumerate(POOL_WINDOWS):
        lo = jnp.clip(t - w // 2, 0, L)
        hi = jnp.clip(t + w - w // 2, 0, L)
        cs = csum[:, :, gi]
        mean = (cs[:, hi] - cs[:, lo]) / (hi - lo).astype(jnp.float32)[None, :, None]
        outs.append(mean - pg[:, :, gi])
    return jnp.stack(outs, axis=2)


def conv_pool_mixer(hc, hl, w_in, w_out, dw, ln_w, ln_b, pool_w, pool_scale):
    B, n_ctx, _ = hc.shape
    h = jnp.concatenate([hc, hl], axis=1)
    L = h.shape[1]
    z = h @ w_in
    a, b, p = z[..., :CONV_W], z[..., CONV_W:2 * CONV_W], z[..., 2 * CONV_W:]
    u = a * jax.nn.sigmoid(b)
    u = jnp.concatenate([depthwise_conv(u[:, :n_ctx], dw), depthwise_conv(u[:, n_ctx:], dw)], axis=1)
    u32 = u.astype(jnp.float32)
    mu = jnp.mean(u32, axis=-1, keepdims=True)
    var = jnp.mean(jnp.square(u32 - mu), axis=-1, keepdims=True)
    u = jax.nn.silu((u32 - mu) * lax.rsqrt(var + EPS) * ln_w.astype(jnp.float32)
                    + ln_b.astype(jnp.float32)).astype(h.dtype)
    pooled = jnp.concatenate([multiscale_pool(p[:, :n_ctx]), multiscale_pool(p[:, n_ctx:])], axis=1)
    pm = jnp.einsum('blgc,gcd->blgd', pooled, pool_w.astype(jnp.float32)).reshape(B, L, POOL_W)
    pm = (pm * pool_scale.astype(jnp.float32)).astype(h.dtype)
    y = jnp.concatenate([u, pm], axis=-1) @ w_out
    return y[:, :n_ctx], y[:, n_ctx:]


def peer_ffn(h, q_w, keys, u, v):
    T, D = h.shape
    q = (h @ q_w).astype(jnp.float32).reshape(T, PEER_HEADS, 2, PEER_DKEY // 2)
    s = jnp.einsum('thpd,hpkd->thpk', q, keys.astype(jnp.float32))
    top_s, top_i = lax.top_k(s, PEER_TOPK)
    cand_s = top_s[:, :, 0, :, None] + top_s[:, :, 1, None, :]
    cand_i = top_i[:, :, 0, :, None] * PEER_KEYS + top_i[:, :, 1, None, :]
    best_s, best_j = lax.top_k(cand_s.reshape(T, PEER_HEADS, PEER_TOPK * PEER_TOPK), PEER_TOPK)
    idx = jnp.take_along_axis(cand_i.reshape(T, PEER_HEADS, PEER_TOPK * PEER_TOPK), best_j, axis=-1)
    gate = jax.nn.softmax(best_s, axis=-1)
    hk = PEER_HEADS * PEER_TOPK
    nb = T // PEER_BLOCK

    def block(args):
        hb, ib, gb = args
        act = jax.nn.gelu(jnp.einsum('td,ted->te', hb, u[ib]), approximate=False)
        return jnp.einsum('te,ted->td', gb * act, v[ib])

    out = lax.map(block, (h.reshape(nb, PEER_BLOCK, D),
                          idx.reshape(nb, PEER_BLOCK, hk),
                          gate.reshape(nb, PEER_BLOCK, hk).astype(h.dtype)))
    return out.reshape(T, D)


def setup_inputs(seed: int = 0) -> dict:
    key = jax.random.key(seed)
    ks = jax.random.split(key, 24)
    D = D_MODEL

    def nrm(k, shape, std):
        return std * jax.random.normal(k, shape, jnp.float32)

    return {
        'x': nrm(ks[0], (BATCH, SEQ, D), 1.0),
        'c': nrm(ks[1], (BATCH, D), 1.0),
        'ctx': nrm(ks[2], (BATCH, CTX_LEN, D), 1.0),
        'c_ctx': nrm(ks[3], (D,), 1.0),
        'ada_w': nrm(ks[4], (DEPTH, D, N_MOD * D), 0.5 * D ** -0.5),
        'ada_b': nrm(ks[5], (DEPTH, N_MOD * D), 0.02),
        'norm1_w': 1.0 + nrm(ks[6], (DEPTH, D), 0.05),
        'norm2_w': 1.0 + nrm(ks[7], (DEPTH, D), 0.05),
        'final_norm_w': 1.0 + nrm(ks[8], (D,), 0.05),
        'ev_in_w': nrm(ks[9], (N_EVEN, D, EVEN_IN), D ** -0.5),
        'ev_out_w': nrm(ks[10], (N_EVEN, MIX_W, D), MIX_W ** -0.5),
        'hg_lb_logits': nrm(ks[11], (N_EVEN, HG_W), 0.5),
        'hg_norm_w': 1.0 + nrm(ks[12], (N_EVEN, HG_DV), 0.05),
        'od_in_w': nrm(ks[13], (N_ODD, D, ODD_IN), D ** -0.5),
        'od_out_w': nrm(ks[14], (N_ODD, MIX_W, D), MIX_W ** -0.5),
        'conv_dw': nrm(ks[15], (N_ODD, CONV_K, CONV_W), CONV_K ** -0.5),
        'conv_ln_w': 1.0 + nrm(ks[16], (N_ODD, CONV_W), 0.05),
        'conv_ln_b': nrm(ks[17], (N_ODD, CONV_W), 0.02),
        'pool_w': nrm(ks[18], (N_ODD, POOL_GROUPS, POOL_GROUP_W, POOL_GROUP_W), POOL_GROUP_W ** -0.5),
        'pool_scale': 1.0 + nrm(ks[19], (N_ODD, POOL_W), 0.1),
        'peer_q_w': nrm(ks[20], (DEPTH, D, PEER_HEADS * PEER_DKEY), D ** -0.5),
        'peer_keys': nrm(ks[21], (DEPTH, PEER_HEADS, 2, PEER_KEYS, PEER_DKEY // 2), (PEER_DKEY // 2) ** -0.5),
        'peer_u': nrm(ks[22], (DEPTH, PEER_EXPERTS, D), D ** -0.5),
        'peer_v': nrm(ks[23], (DEPTH, PEER_EXPERTS, D), 0.5),
    }


def reference(x, c, ctx, c_ctx, ada_w, ada_b, norm1_w, norm2_w, final_norm_w,
              ev_in_w, ev_out_w, hg_lb_logits, hg_norm_w,
              od_in_w, od_out_w, conv_dw, conv_ln_w, conv_ln_b, pool_w, pool_scale,
              peer_q_w, peer_keys, peer_u, peer_v):
    B, S, D = x.shape
    n_ctx = ctx.shape[1]
    rows = S // GRID_W
    xl = x + sincos_2d(rows, D).astype(x.dtype)[None]
    xc = ctx
    lb_p = jax.nn.softmax(hg_lb_logits.astype(jnp.float32), axis=0)
    lower_bounds = jnp.concatenate([jnp.zeros_like(lb_p[:1]), jnp.cumsum(lb_p[1:], axis=0)], axis=0)
    lower_bounds = jnp.clip(lower_bounds, 0.0, 1.0)
    sc_l = jax.nn.silu(c)
    sc_c = jax.nn.silu(c_ctx)
    for l in range(DEPTH):
        mod_l = (sc_l @ ada_w[l] + ada_b[l]).reshape(B, 1, N_MOD, D)
        mod_c = (sc_c @ ada_w[l] + ada_b[l]).reshape(N_MOD, D)
        hc = modulate(rms_norm(xc, norm1_w[l]), mod_c[0], mod_c[1])
        hl = modulate(rms_norm(xl, norm1_w[l]), mod_l[:, :, 0], mod_l[:, :, 1])
        j = l // 2
        if l % 2 == 0:
            yc, yl = fourier_hgrn_mixer(hc, hl, ev_in_w[j], ev_out_w[j], lower_bounds[j], hg_norm_w[j])
        else:
            yc, yl = conv_pool_mixer(hc, hl, od_in_w[j], od_out_w[j], conv_dw[j], conv_ln_w[j],
                                     conv_ln_b[j], pool_w[j], pool_scale[j])
        xc = xc + mod_c[2] * yc
        xl = xl + mod_l[:, :, 2] * yl
        hc = modulate(rms_norm(xc, norm2_w[l]), mod_c[3], mod_c[4])
        hl = modulate(rms_norm(xl, norm2_w[l]), mod_l[:, :, 3], mod_l[:, :, 4])
        h_all = jnp.concatenate([hc.reshape(B * n_ctx, D), hl.reshape(B * S, D)], axis=0)
        y_all = peer_ffn(h_all, peer_q_w[l], peer_keys[l], peer_u[l], peer_v[l])
        xc = xc + mod_c[5] * y_all[:B * n_ctx].reshape(B, n_ctx, D)
        xl = xl + mod_l[:, :, 5] * y_all[B * n_ctx:].reshape(B, S, D)
    return rms_norm(xl, final_norm_w)
```

```python
import numpy as np
from contextlib import ExitStack
import concourse.bass as bass
import concourse.mybir as mybir
from concourse.bass_utils import run_bass_kernel_spmd

F32 = mybir.dt.float32
BF16 = mybir.dt.bfloat16
I32 = mybir.dt.int32
U32 = mybir.dt.uint32
ALU = mybir.AluOpType
AF = mybir.ActivationFunctionType
AX = mybir.AxisListType

ENGS = ('sp', 'act', 'dve', 'pe', 'pool')
SEM_LIMIT = 30000
NDMA = 6


class Prog:
    def __init__(self):
        self.nc = bass.Bass("TRN2", target_bir_lowering=False)
        self.es = ExitStack()
        self.ops = {e: [] for e in ENGS}
        self.cnt = {e: 0 for e in ENGS}
        self.last_w = {}
        self.readers = {}
        self.seen = {e: {} for e in ENGS}
        self.dma_val = {}
        self.dma_next = {e: 0 for e in ENGS}
        self.semkeys = set()
        self.n_t = 0

    def dram(self, name, shape, dtype, kind):
        return self.nc.dram_tensor(name, list(shape), dtype, kind=kind).ap()

    def inp(self, name, shape, dtype=F32):
        return self.dram(name, shape, dtype, "ExternalInput")

    def outp(self, name, shape, dtype=F32):
        return self.dram(name, shape, dtype, "ExternalOutput")

    def scratch(self, name, shape, dtype=F32):
        return self.dram(name, shape, dtype, "Internal")

    def sb(self, name, shape, dtype=F32):
        return self.es.enter_context(self.nc.sbuf_tensor(name, list(shape), dtype))

    def ps(self, name, shape=(128, 512), dtype=F32):
        return self.es.enter_context(self.nc.psum_tensor(name, list(shape), dtype))

    def _deps(self, eng, r, w):
        toks = set()
        for k in r:
            t = self.last_w.get(k)
            if t is not None:
                toks.add(t)
        for k in w:
            t = self.last_w.get(k)
            if t is not None:
                toks.add(t)
            for t in self.readers.get(k, ()):
                toks.add(t)
        waits = {}
        for (key, val) in toks:
            if eng == 'pe' and key[0] == 'pe':
                continue
            if self.seen[eng].get(key, 0) >= val:
                continue
            waits[key] = max(waits.get(key, 0), val)
        for key, val in waits.items():
            self.seen[eng][key] = val
        return list(waits.items())

    def _commit(self, tok, r, w):
        for k in r:
            self.readers.setdefault(k, []).append(tok)
        for k in w:
            self.last_w[k] = tok
            self.readers[k] = []

    def op(self, eng, fn, r=(), w=()):
        waits = self._deps(eng, r, w)
        self.cnt[eng] += 1
        c = self.cnt[eng]
        key = (eng, (c - 1) // SEM_LIMIT)
        val = (c - 1) % SEM_LIMIT + 1
        self.semkeys.add(key)
        self.ops[eng].append((waits, fn, key, 1))
        self._commit((key, val), r, w)

    def dma(self, eng, fn, r=(), w=()):
        i = self.dma_next[eng]
        self.dma_next[eng] = (i + 1) % NDMA
        key = ('d' + eng, i)
        self.semkeys.add(key)
        prev = self.dma_val.get(key, 0)
        waits = self._deps(eng, r, w)
        if prev > 0 and self.seen[eng].get(key, 0) < prev:
            waits.append((key, prev))
            self.seen[eng][key] = prev
        val = prev + 16
        self.dma_val[key] = val
        self.ops[eng].append((waits, fn, key, 16))
        self._commit((key, val), r, w)

    def mm(self, out, lhsT, rhs, start, stop, r, w):
        self.op('pe', lambda e: e.matmul(out, lhsT, rhs, start=start, stop=stop), r, w)

    def tr(self, out, in_, ident, r, w):
        self.op('pe', lambda e: e.transpose(out, in_, ident), r, w)

    def act(self, out, in_, func, r, w, bias=None, scale=None, accum_out=None):
        kw = {}
        if bias is not None:
            kw['bias'] = bias
        if scale is not None:
            kw['scale'] = scale
        if accum_out is not None:
            kw['accum_out'] = accum_out
        self.op('act', lambda e: e.activation(out, in_, func, **kw), r, w)

    def tt(self, out, in0, in1, op, r, w, eng='dve'):
        self.op(eng, lambda e: e.tensor_tensor(out, in0, in1, op), r, w)

    def ts(self, out, in0, s1, s2, op0, op1, r, w, eng='dve', accum_out=None):
        if accum_out is not None:
            self.op(eng, lambda e: e.tensor_scalar(out, in0, s1, s2, op0, op1, accum_out), r, w)
        elif op1 is None:
            self.op(eng, lambda e: e.tensor_scalar(out, in0, s1, None, op0), r, w)
        else:
            self.op(eng, lambda e: e.tensor_scalar(out, in0, s1, s2, op0, op1), r, w)

    def stt(self, out, in0, scalar, in1, op0, op1, r, w, eng='dve'):
        self.op(eng, lambda e: e.scalar_tensor_tensor(out, in0, scalar, in1, op0, op1), r, w)

    def cp(self, out, in_, r, w, eng='dve'):
        if eng == 'act':
            self.op(eng, lambda e: e.copy(out, in_), r, w)
        else:
            self.op(eng, lambda e: e.tensor_copy(out, in_), r, w)

    def memset(self, ap, val, w, eng='dve'):
        self.op(eng, lambda e: e.memset(ap, val), (), w)

    def ld(self, out, in_, r, w, eng='sp'):
        self.dma(eng, lambda e: e.dma_start(out=out, in_=in_), r, w)

    def build(self):
        nc = self.nc
        fin = []
        for key, val in self.dma_val.items():
            if self.seen['sp'].get(key, 0) < val:
                fin.append((key, val))
        for e in ENGS:
            c = self.cnt[e]
            if c > 0:
                key = (e, (c - 1) // SEM_LIMIT)
                val = (c - 1) % SEM_LIMIT + 1
                if e != 'sp' and self.seen['sp'].get(key, 0) < val:
                    fin.append((key, val))
        sems = {}
        for key in sorted(self.semkeys, key=str):
            nm = "s_%s_%s" % (key[0], key[1])
            sems[key] = self.es.enter_context(nc.semaphore(nm))
        ops = self.ops

        def replay(ename, eng):
            for (waits, fn, key, inc) in ops[ename]:
                for (k, v) in waits:
                    eng.wait_ge(sems[k], v)
                ins = fn(eng)
                ins.then_inc(sems[key], inc)

        with nc.Block() as block:
            @block.sync
            def _(e):
                replay('sp', e)
                for (k, v) in fin:
                    e.wait_ge(sems[k], v)

            @block.scalar
            def _(e):
                replay('act', e)

            @block.vector
            def _(e):
                replay('dve', e)

            @block.tensor
            def _(e):
                replay('pe', e)

            @block.gpsimd
            def _(e):
                replay('pool', e)
        self.es.close()
        return nc

    def n_ins(self):
        return {e: len(self.ops[e]) for e in ENGS}


def build_mod():
    P = Prog()
    cT = P.inp("cT", [128, 16, 3])
    w = P.inp("w", [2048, 6144])
    b = P.inp("b", [1, 6144])
    out = P.outp("mod", [3, 6144])
    c_sb = P.sb("c_sb", [128, 16, 3])
    sc = P.sb("sc", [128, 16, 3])
    b_sb = P.sb("b_sb", [1, 6144])
    ones = P.sb("ones", [1, 4])
    o_sb = P.sb("o_sb", [3, 6144])
    wb = [P.sb("wb%d" % i, [128, 16, 512]) for i in range(2)]
    pss = [P.ps("ps%d" % i) for i in range(2)]
    P.ld(c_sb[:], cT, [], ['c_sb'])
    P.ld(b_sb[:], b, [], ['b_sb'])
    P.memset(ones[:], 1.0, ['ones'])
    P.act(sc[:], c_sb[:], AF.Silu, ['c_sb'], ['sc'])
    wv = w.rearrange("(k p) n -> p k n", p=128)
    for nt in range(12):
        wbuf = wb[nt % 2]
        ps = pss[nt % 2]
        P.ld(wbuf[:], wv[:, :, nt * 512:(nt + 1) * 512], [], ['wb%d' % (nt % 2)])
        for k in range(16):
            P.mm(ps[0:3, :], sc[:, k, :], wbuf[:, k, :], k == 0, False, ['sc', 'wb%d' % (nt % 2)], ['ps%d' % (nt % 2)])
        P.mm(ps[0:3, :], ones[0:1, 0:3], b_sb[0:1, nt * 512:(nt + 1) * 512], False, True, ['ones', 'b_sb'], ['ps%d' % (nt % 2)])
        P.cp(o_sb[:, nt * 512:(nt + 1) * 512], ps[0:3, :], ['ps%d' % (nt % 2)], ['o_sb'])
    P.ld(out, o_sb[:], ['o_sb'], ['out'])
    return P


NEG = -1.0e30


def build_peer(tiles, final_norm=False, nug=3):
    T = sum(tiles)
    P = Prog()
    h2 = P.inp("h2", [T, 2048])
    x1 = P.inp("x1", [T, 2048])
    qw = P.inp("qw", [2048, 2048])
    keysT = P.inp("keysT", [128, 16, 128])
    u = P.inp("u", [16384, 2048])
    v = P.inp("v", [16384, 2048])
    g5 = P.inp("g5", [2, 2048])
    fnw = P.inp("fnw", [1, 2048])
    identd = P.inp("ident", [128, 128])
    iotad = P.inp("iota16", [128, 16])
    out = P.outp("x2", [T, 2048])

    ident = P.sb("ident_sb", [128, 128])
    identb = P.sb("identb", [128, 128], BF16)
    iota = P.sb("iota_sb", [128, 16])
    colsel = P.sb("colsel", [128, 256], BF16)
    qwb = P.sb("qwb", [128, 16, 2048], BF16)
    stg = [P.sb("stg%d" % i, [128, 2048]) for i in range(2)]
    keysb = P.sb("keysb", [128, 16, 128], BF16)
    g5bc = P.sb("g5bc", [128, 2048])
    fnbc = P.sb("fnbc", [128, 2048])
    h2s = P.sb("h2s", [128, 2048])
    h2b = P.sb("h2b", [128, 2048], BF16)
    hT = P.sb("hT", [128, 16, 128], BF16)
    qT = P.sb("qT", [128, 16, 128], BF16)
    s_sb = P.sb("s_sb", [128, 2048])
    sw = P.sb("sw", [128, 256])
    tops = P.sb("tops", [128, 16, 16])
    topi = P.sb("topi", [128, 16, 16], U32)
    topif = P.sb("topif", [128, 16, 16])
    cand = P.sb("cand", [128, 8, 256])
    best = P.sb("best", [128, 8, 16])
    bpos = P.sb("bpos", [128, 8, 16], U32)
    hi_u = P.sb("hi_u", [128, 128], U32)
    lo_u = P.sb("lo_u", [128, 128], U32)
    hif = P.sb("hif", [128, 8, 16])
    lof = P.sb("lof", [128, 8, 16])
    oh = P.sb("oh", [128, 16, 16])
    sel0 = P.sb("sel0", [128, 8, 16])
    sel1 = P.sb("sel1", [128, 8, 16])
    idxf = P.sb("idxf", [128, 128])
    gate = P.sb("gate", [128, 8, 16])
    gsum = P.sb("gsum", [128, 8])
    idxT = P.sb("idxT", [128, 128], I32)
    gateT = P.sb("gateT", [128, 128])
    actT = P.sb("actT", [128, 128])
    coefT = P.sb("coefT", [128, 128])
    junk = P.sb("junk", [128, 2048], BF16)
    Ug = [P.sb("Ug%d" % i, [128, 2048], BF16) for i in range(nug)]
    Vg = [P.sb("Vg%d" % i, [128, 2048], BF16) for i in range(nug)]
    Z = [P.sb("Z%d" % i, [128, 128], BF16) for i in range(2)]
    xs = P.sb("xs", [128, 2048])
    xo = P.sb("xo", [128, 2048])
    ssq = P.sb("ssq", [128, 1])
    rstd = P.sb("rstd", [128, 1])
    psA = P.ps("psA", [128, 2048])
    psB = P.ps("psB", [128, 2048])
    A = ['psA0', 'psA1', 'psA2', 'psA3']

    P.ld(ident[:], identd, [], ['ident'])
    P.ld(iota[:], iotad, [], ['iota'])
    P.cp(identb[:], ident[:], ['ident'], ['identb'])
    P.memset(colsel[:], 0.0, ['colsel'])
    P.memset(colsel[:, 128:129], 1.0, ['colsel'])
    qv = qw.rearrange("(k p) n -> p k n", p=128)
    for k in range(16):
        sname = 'stg%d' % (k % 2)
        P.ld(stg[k % 2][:], qv[:, k, :], [], [sname])
        P.cp(qwb[:, k, :], stg[k % 2][:], [sname], ['qwb'], eng='pool')
    P.ld(stg[0][:, 0:2048], keysT.rearrange("p j k -> p (j k)"), [], ['stg0'])
    P.cp(keysb[:].rearrange("p j k -> p (j k)"), stg[0][:], ['stg0'], ['keysb'])
    P.ld(fnbc[:], fnw.partition_broadcast(128), [], ['fnbc'])

    def tile_body(ti, nt, t0):
        if ti <= 1:
            row = 0 if ti == 0 else 1
            P.ld(g5bc[:], g5[row:row + 1, :].partition_broadcast(128), [], ['g5bc'])
        P.ld(h2s[:nt, :], h2[t0:t0 + nt, :], [], ['h2s'])
        P.ld(xs[:nt, :], x1[t0:t0 + nt, :], [], ['xs'])
        P.cp(h2b[:nt, :], h2s[:nt, :], ['h2s'], ['h2b'], eng='pool')
        for k in range(16):
            bk = k % 4
            P.tr(psA[:, bk * 512:bk * 512 + nt], h2s[:nt, k * 128:(k + 1) * 128], ident[:nt, :nt], ['h2s', 'ident'], [A[bk]])
            P.op('act', lambda e, k=k, bk=bk: e.copy(hT[:, k, :nt], psA[:, bk * 512:bk * 512 + nt]), [A[bk]], ['hT'])
        for j in range(16):
            bk = j % 4
            for k in range(16):
                P.mm(psA[:, bk * 512:bk * 512 + nt], qwb[:, k, j * 128:(j + 1) * 128], hT[:, k, :nt], k == 0, k == 15,
                     ['qwb', 'hT'], [A[bk]])
            P.op('act', lambda e, j=j, bk=bk: e.copy(qT[:, j, :nt], psA[:, bk * 512:bk * 512 + nt]), [A[bk]], ['qT'])
        for j in range(16):
            bk = j // 4
            P.mm(psA[:nt, j * 128:(j + 1) * 128], qT[:, j, :nt], keysb[:, j, :], True, True, ['qT', 'keysb'], [A[bk]])
        for bk in range(4):
            P.op('act', lambda e, bk=bk: e.copy(s_sb[:nt, bk * 512:(bk + 1) * 512], psA[:nt, bk * 512:(bk + 1) * 512]),
                 [A[bk]], ['s_sb'])
        for j in range(16):
            sj = s_sb[:nt, j * 128:(j + 1) * 128]
            P.op('dve', lambda e, j=j, sj=sj: e.max(tops[:nt, j, 0:8], sj), ['s_sb'], ['tops'])
            P.op('dve', lambda e, j=j, sj=sj: e.max_index(topi[:nt, j, 0:8], tops[:nt, j, 0:8], sj), ['s_sb', 'tops'], ['topi'])
            P.op('dve', lambda e, j=j, sj=sj: e.match_replace(sw[:nt, 0:128], tops[:nt, j, 0:8], sj, NEG), ['s_sb', 'tops'], ['sw'])
            P.op('dve', lambda e, j=j: e.max(tops[:nt, j, 8:16], sw[:nt, 0:128]), ['sw'], ['tops'])
            P.op('dve', lambda e, j=j: e.max_index(topi[:nt, j, 8:16], tops[:nt, j, 8:16], sw[:nt, 0:128]), ['sw', 'tops'], ['topi'])
        P.cp(topif[:nt], topi[:nt], ['topi'], ['topif'])
        for h in range(8):
            c3 = cand[:nt, h, :].rearrange("t (a b) -> t a b", b=16)
            a0 = tops[:nt, 2 * h, :].unsqueeze(2).to_broadcast([nt, 16, 16])
            a1 = tops[:nt, 2 * h + 1, :].unsqueeze(1).to_broadcast([nt, 16, 16])
            P.tt(c3, a0, a1, ALU.add, ['tops'], ['cand'])
            cs = cand[:nt, h, :]
            P.op('dve', lambda e, h=h, cs=cs: e.max(best[:nt, h, 0:8], cs), ['cand'], ['best'])
            P.op('dve', lambda e, h=h, cs=cs: e.max_index(bpos[:nt, h, 0:8], best[:nt, h, 0:8], cs), ['cand', 'best'], ['bpos'])
            P.op('dve', lambda e, h=h, cs=cs: e.match_replace(sw[:nt, :], best[:nt, h, 0:8], cs, NEG), ['cand', 'best'], ['sw'])
            P.op('dve', lambda e, h=h: e.max(best[:nt, h, 8:16], sw[:nt, :]), ['sw'], ['best'])
            P.op('dve', lambda e, h=h: e.max_index(bpos[:nt, h, 8:16], best[:nt, h, 8:16], sw[:nt, :]), ['sw', 'best'], ['bpos'])
        bp2 = bpos[:nt].rearrange("t h r -> t (h r)")
        P.op('dve', lambda e: e.tensor_single_scalar(hi_u[:nt, :], bp2, 4, ALU.logical_shift_right), ['bpos'], ['hi_u'])
        P.op('dve', lambda e: e.tensor_single_scalar(lo_u[:nt, :], bp2, 15, ALU.bitwise_and), ['bpos'], ['lo_u'])
        P.cp(hif[:nt].rearrange("t h r -> t (h r)"), hi_u[:nt, :], ['hi_u'], ['hif'])
        P.cp(lof[:nt].rearrange("t h r -> t (h r)"), lo_u[:nt, :], ['lo_u'], ['lof'])
        io3 = iota[:nt, :].unsqueeze(1).to_broadcast([nt, 16, 16])
        for h in range(8):
            for (srcf, jj, dst, dn) in ((hif, 2 * h, sel0, 'sel0'), (lof, 2 * h + 1, sel1, 'sel1')):
                P.tt(oh[:nt], io3, srcf[:nt, h, :].unsqueeze(2).to_broadcast([nt, 16, 16]), ALU.is_equal, ['iota', 'hif', 'lof'], ['oh'])
                P.tt(oh[:nt], oh[:nt], topif[:nt, jj, :].unsqueeze(1).to_broadcast([nt, 16, 16]), ALU.mult, ['oh', 'topif'], ['oh'])
                P.op('dve', lambda e, dst=dst, h=h: e.reduce_sum(dst[:nt, h, :], oh[:nt], AX.X), ['oh'], [dn])
        P.stt(idxf[:nt, :], sel0[:nt].rearrange("t h r -> t (h r)"), 128.0, sel1[:nt].rearrange("t h r -> t (h r)"),
              ALU.mult, ALU.add, ['sel0', 'sel1'], ['idxf'])
        P.tt(gate[:nt], best[:nt], best[:nt, :, 0:1].to_broadcast([nt, 8, 16]), ALU.subtract, ['best'], ['gate'])
        P.act(gate[:nt], gate[:nt], AF.Exp, ['gate'], ['gate'])
        P.op('dve', lambda e: e.reduce_sum(gsum[:nt, :], gate[:nt], AX.X), ['gate'], ['gsum'])
        P.op('dve', lambda e: e.reciprocal(gsum[:nt, :], gsum[:nt, :]), ['gsum'], ['gsum'])
        P.tt(gate[:nt], gate[:nt], gsum[:nt, :].unsqueeze(2).to_broadcast([nt, 8, 16]), ALU.mult, ['gate', 'gsum'], ['gate'])
        P.tr(psA[:, 0:nt], idxf[:nt, :], ident[:nt, :nt], ['idxf', 'ident'], [A[0]])
        P.cp(idxT[:, :nt], psA[:, 0:nt], [A[0]], ['idxT'])
        P.tr(psA[:, 512:512 + nt], gate[:nt].rearrange("t h r -> t (h r)"), ident[:nt, :nt], ['gate', 'ident'], [A[1]])
        P.cp(gateT[:, :nt], psA[:, 512:512 + nt], [A[1]], ['gateT'])
        for t in range(nt):
            b = t % nug
            P.dma('pool', lambda e, b=b, t=t: e.indirect_dma_start(
                out=Ug[b][:], out_offset=None, in_=u,
                in_offset=bass.IndirectOffsetOnAxis(ap=idxT[:, t:t + 1], axis=0)), ['idxT'], ['Ug%d' % b])
            for n in range(4):
                P.mm(psB[:, n * 512:(n + 1) * 512], identb[:nt, t:t + 1].to_broadcast([nt, 128]), h2b[:nt, n * 512:(n + 1) * 512],
                     True, True, ['identb', 'h2b'], ['psB'])
            P.op('dve', lambda e, b=b, t=t: e.scalar_tensor_tensor(
                junk[:], Ug[b][:], 1.0, psB[:], ALU.mult, ALU.mult, actT[:, t:t + 1]),
                ['Ug%d' % b, 'psB'], ['junk', 'actT'])
        P.act(coefT[:, :nt], actT[:, :nt], AF.Gelu, ['actT'], ['coefT'])
        P.tt(coefT[:, :nt], coefT[:, :nt], gateT[:, :nt], ALU.mult, ['coefT', 'gateT'], ['coefT'])
        for t in range(nt):
            b = t % nug
            zb = t % 2
            P.dma('pool', lambda e, b=b, t=t: e.indirect_dma_start(
                out=Vg[b][:], out_offset=None, in_=v,
                in_offset=bass.IndirectOffsetOnAxis(ap=idxT[:, t:t + 1], axis=0)), ['idxT'], ['Vg%d' % b])
            P.ts(Z[zb][:, :], colsel[:, 128 - t:256 - t], coefT[:, t:t + 1], None, ALU.mult, None, ['colsel', 'coefT'], ['Z%d' % zb])
            for n in range(4):
                P.mm(psA[:nt, n * 512:(n + 1) * 512], Z[zb][:, :nt], Vg[b][:, n * 512:(n + 1) * 512], t == 0, t == nt - 1,
                     ['Z%d' % zb, 'Vg%d' % b], [A[n]])
        P.tt(xo[:nt, :], psA[:nt, :], g5bc[:nt, :], ALU.mult, A + ['g5bc'], ['xo'])
        P.tt(xo[:nt, :], xo[:nt, :], xs[:nt, :], ALU.add, ['xo', 'xs'], ['xo'])
        if final_norm:
            P.act(junk[:nt, :], xo[:nt, :], AF.Square, ['xo'], ['junk', 'ssq'], accum_out=ssq[:nt, :])
            P.ts(ssq[:nt, :], ssq[:nt, :], 1.0 / 2048, 1e-6, ALU.mult, ALU.add, ['ssq'], ['ssq'])
            P.act(ssq[:nt, :], ssq[:nt, :], AF.Sqrt, ['ssq'], ['ssq'])
            P.op('dve', lambda e: e.reciprocal(rstd[:nt, :], ssq[:nt, :]), ['ssq'], ['rstd'])
            P.stt(xo[:nt, :], xo[:nt, :], rstd[:nt, 0:1], fnbc[:nt, :], ALU.mult, ALU.mult, ['xo', 'rstd', 'fnbc'], ['xo'])
        P.ld(out[t0:t0 + nt, :], xo[:nt, :], ['xo'], ['out'])

    t0 = 0
    for ti, nt in enumerate(tiles):
        tile_body(ti, nt, t0)
        t0 += nt
    return P


def peer_consts():
    return {"ident": np.eye(128, dtype=np.float32),
            "iota16": np.tile(np.arange(16, dtype=np.float32)[None, :], (128, 1))}


def peer_ref(h2, x1, qw, keys, u, v, g5rows, fnw, final_norm):
    T = h2.shape[0]
    q = (h2 @ qw).reshape(T, 8, 2, 128)
    s = np.einsum('thpd,hpkd->thpk', q, keys)
    ti = np.argsort(-s, axis=-1)[..., :16]
    ts_ = np.take_along_axis(s, ti, -1)
    cs = ts_[:, :, 0, :, None] + ts_[:, :, 1, None, :]
    ci = ti[:, :, 0, :, None] * 128 + ti[:, :, 1, None, :]
    cs = cs.reshape(T, 8, 256)
    ci = ci.reshape(T, 8, 256)
    bj = np.argsort(-cs, axis=-1)[..., :16]
    bs = np.take_along_axis(cs, bj, -1)
    idx = np.take_along_axis(ci, bj, -1)
    g = np.exp(bs - bs.max(-1, keepdims=True))
    g = g / g.sum(-1, keepdims=True)
    idx = idx.reshape(T, 128)
    g = g.reshape(T, 128)
    from scipy.special import erf
    y = np.zeros((T, 2048), np.float32)
    for t in range(T):
        a = u[idx[t]] @ h2[t]
        a = 0.5 * a * (1 + erf(a / np.sqrt(2)))
        y[t] = (g[t] * a) @ v[idx[t]]
    xo = x1 + g5rows * y
    if final_norm:
        xo = xo / np.sqrt((xo * xo).mean(-1, keepdims=True) + 1e-6) * fnw
    return xo, idx, g


def build_pre(ttiles, N):
    T = sum(ttiles)
    P = Prog()
    xT = P.inp("xT", [2048, T])
    w = P.inp("w", [2048, N])
    nwd = P.inp("nw", [128, 16])
    modd = P.inp("mods", [128, 2, 2, 16])
    zT = P.outp("zT", [N, T])
    ones = P.sb("ones", [128, 128])
    nw = P.sb("nw_sb", [128, 16])
    mods = P.sb("mods_sb", [128, 2, 2, 16])
    gg = P.sb("gg", [128, 2, 16])
    xt = P.sb("xt", [128, 16, 512])
    sq = [P.sb("sq%d" % i, [128, 512]) for i in range(2)]
    rstd = P.sb("rstd", [128, 512])
    tmp = [P.sb("tmp%d" % i, [128, 512]) for i in range(2)]
    hT = P.sb("hT", [128, 16, T], BF16)
    CG = 256
    wst = [P.sb("wst%d" % i, [128, 16, CG]) for i in range(2)]
    wbf = [P.sb("wbf%d" % i, [128, 16, CG], BF16) for i in range(2)]
    ostg = [P.sb("ostg%d" % i, [128, T]) for i in range(2)]
    pss = P.ps("pss")
    psm = [P.ps("psm%d" % i) for i in range(4)]
    P.memset(ones[:], 1.0, ['ones'])
    P.ld(nw[:], nwd, [], ['nw'])
    P.ld(mods[:], modd, [], ['mods'])
    for r in range(2):
        P.ts(gg[:, r, :], mods[:, r, 1, :], 1.0, None, ALU.add, None, ['mods'], ['gg'])
        P.tt(gg[:, r, :], gg[:, r, :], nw[:], ALU.mult, ['gg', 'nw'], ['gg'])
    xv = xT.rearrange("(k p) t -> p k t", p=128)

    def tile_body(ti, nt, t0):
        r = 0 if ti == 0 else 1
        P.ld(xt[:, :, :nt], xv[:, :, t0:t0 + nt], [], ['xt'])
        for k in range(16):
            s = sq[k % 2]
            P.act(s[:, :nt], xt[:, k, :nt], AF.Square, ['xt'], ['sq%d' % (k % 2)])
            P.mm(pss[:, :nt], ones[:], s[:, :nt], k == 0, k == 15, ['ones', 'sq%d' % (k % 2)], ['pss'])
        P.ts(rstd[:, :nt], pss[:, :nt], 1.0 / 2048, 1e-6, ALU.mult, ALU.add, ['pss'], ['rstd'])
        P.act(rstd[:, :nt], rstd[:, :nt], AF.Sqrt, ['rstd'], ['rstd'])
        P.op('dve', lambda e: e.reciprocal(rstd[:, :nt], rstd[:, :nt]), ['rstd'], ['rstd'])
        for k in range(16):
            tm = tmp[k % 2]
            eng = 'dve' if k % 2 == 0 else 'pool'
            P.tt(tm[:, :nt], xt[:, k, :nt], rstd[:, :nt], ALU.mult, ['xt', 'rstd'], ['tmp%d' % (k % 2)], eng=eng)
            P.ts(hT[:, k, t0:t0 + nt], tm[:, :nt], gg[:, r, k:k + 1], mods[:, r, 0, k:k + 1], ALU.mult, ALU.add,
                 ['tmp%d' % (k % 2), 'gg', 'mods'], [('hT', ti)], eng=eng)

    t0 = 0
    for ti, nt in enumerate(ttiles):
        tile_body(ti, nt, t0)
        t0 += nt
    hall = [('hT', ti) for ti in range(len(ttiles))]
    wv = w.rearrange("(k p) n -> p k n", p=128)
    ncg = N // CG
    cnt = 0
    for cg in range(ncg):
        wb = cg % 2
        P.ld(wst[wb][:], wv[:, :, cg * CG:(cg + 1) * CG], [], ['wst%d' % wb])
        P.cp(wbf[wb][:, 0:8, :], wst[wb][:, 0:8, :], ['wst%d' % wb], ['wbf%d' % wb], eng='pool')
        P.cp(wbf[wb][:, 8:16, :], wst[wb][:, 8:16, :], ['wst%d' % wb], ['wbf%d' % wb], eng='act')
        for cb in range(CG // 128):
            ob = (cg * (CG // 128) + cb) % 2
            t0 = 0
            for ti, nt in enumerate(ttiles):
                pb = cnt % 4
                cnt += 1
                for k in range(16):
                    P.mm(psm[pb][:, :nt], wbf[wb][:, k, cb * 128:(cb + 1) * 128], hT[:, k, t0:t0 + nt], k == 0, k == 15,
                         ['wbf%d' % wb, ('hT', ti)], ['psm%d' % pb])
                if cnt % 2 == 0:
                    P.cp(ostg[ob][:, t0:t0 + nt], psm[pb][:, :nt], ['psm%d' % pb], ['ostg%d' % ob])
                else:
                    P.op('act', lambda e, ob=ob, pb=pb, t0=t0, nt=nt: e.copy(ostg[ob][:, t0:t0 + nt], psm[pb][:, :nt]),
                         ['psm%d' % pb], ['ostg%d' % ob])
                t0 += nt
            c0 = cg * CG + cb * 128
            P.ld(zT[c0:c0 + 128, :], ostg[ob][:], ['ostg%d' % ob], ['zT'], eng='act')
    return P


def build_A(tiles, even):
    T = sum(tiles)
    P = Prog()
    if even:
        fourT = P.inp("fourT", [1024, T])
        ofT = P.inp("ofT", [1024, T])
        obT = P.inp("obT", [1024, T])
        gT = P.inp("gT", [1024, T])
        hnwd = P.inp("hnw", [128, 1])
    else:
        mTd = P.inp("mT", [2048, T])
    x = P.inp("x", [T, 2048])
    w = P.inp("w", [2048, 2048])
    rows = P.inp("rows", [2, 4, 2048])
    x1o = P.outp("x1", [T, 2048])
    h2o = P.outp("h2", [T, 2048])
    wb = P.sb("wb", [128, 16, 2048], BF16)
    stg = [P.sb("stg%d" % i, [128, 2048]) for i in range(2)]
    bc = P.sb("bc", [128, 4, 2048])
    ms = P.sb("ms", [128, 16, 128])
    mb = P.sb("mb", [128, 16, 128], BF16)
    xs = P.sb("xs", [128, 2048])
    x1 = P.sb("x1s", [128, 2048])
    h2 = P.sb("h2s", [128, 2048])
    junk = P.sb("junk", [128, 2048])
    ssq = P.sb("ssq", [128, 1])
    rstd = P.sb("rstd", [128, 1])
    psA = P.ps("psA", [128, 2048])
    A = ['psA0', 'psA1', 'psA2', 'psA3']
    if even:
        ones = P.sb("ones", [128, 128])
        hnw = P.sb("hnw_sb", [128, 1])
        o1 = P.sb("o1", [128, 8, 128])
        o2 = P.sb("o2", [128, 8, 128])
        gs = P.sb("gs", [128, 8, 128])
        osq = P.sb("osq", [128, 8, 128])
        orst = P.sb("orst", [128, 8, 128])
        psO = P.ps("psO", [128, 1024])
        P.memset(ones[:], 1.0, ['ones'])
        P.ld(hnw[:], hnwd, [], ['hnw'])
    wv = w.rearrange("(k p) n -> p k n", p=128)
    for k in range(16):
        P.ld(stg[k % 2][:], wv[:, k, :], [], ['stg%d' % (k % 2)])
        P.cp(wb[:, k, :], stg[k % 2][:], ['stg%d' % (k % 2)], ['wb'], eng='pool' if k % 2 else 'act')

    def tile_body(ti, nt, t0):
        if ti <= 1:
            for q in range(4):
                P.ld(bc[:, q, :], rows[ti, q:q + 1, :].partition_broadcast(128), [], ['bc'])
            P.ts(bc[:, 2, :], bc[:, 2, :], 1.0, None, ALU.add, None, ['bc'], ['bc'])
            P.tt(bc[:, 2, :], bc[:, 2, :], bc[:, 1, :], ALU.mult, ['bc'], ['bc'])
        if even:
            P.ld(ms[:, 0:8, :nt], fourT.rearrange("(k p) t -> p k t", p=128)[:, :, t0:t0 + nt], [], ['ms'])
            P.ld(o1[:, :, :nt], ofT.rearrange("(k p) t -> p k t", p=128)[:, :, t0:t0 + nt], [], ['o1'])
            P.ld(o2[:, :, :nt], obT.rearrange("(k p) t -> p k t", p=128)[:, :, t0:t0 + nt], [], ['o2'])
            P.ld(gs[:, :, :nt], gT.rearrange("(k p) t -> p k t", p=128)[:, :, t0:t0 + nt], [], ['gs'])
            P.tt(o1[:, :, :nt], o1[:, :, :nt], o2[:, :, :nt], ALU.add, ['o1', 'o2'], ['o1'])
            P.act(osq[:, :, :nt], o1[:, :, :nt], AF.Square, ['o1'], ['osq'])
            for hh in range(8):
                P.mm(psO[:, hh * 128:hh * 128 + nt], ones[:], osq[:, hh, :nt], True, True, ['ones', 'osq'], ['psO'])
            for hh in range(8):
                P.ts(orst[:, hh, :nt], psO[:, hh * 128:hh * 128 + nt], 1.0 / 128, 1e-6, ALU.mult, ALU.add, ['psO'], ['orst'])
            P.act(orst[:, :, :nt], orst[:, :, :nt], AF.Sqrt, ['orst'], ['orst'])
            P.op('dve', lambda e: e.reciprocal(orst[:, :, :nt], orst[:, :, :nt]), ['orst'], ['orst'])
            P.act(gs[:, :, :nt], gs[:, :, :nt], AF.Silu, ['gs'], ['gs'])
            P.tt(o1[:, :, :nt], o1[:, :, :nt], orst[:, :, :nt], ALU.mult, ['o1', 'orst'], ['o1'])
            for hh in range(8):
                P.stt(ms[:, 8 + hh, :nt], o1[:, hh, :nt], hnw[:, 0:1], gs[:, hh, :nt], ALU.mult, ALU.mult,
                      ['o1', 'hnw', 'gs'], ['ms'])
        else:
            P.ld(ms[:, :, :nt], mTd.rearrange("(k p) t -> p k t", p=128)[:, :, t0:t0 + nt], [], ['ms'])
        P.cp(mb[:, :, :nt], ms[:, :, :nt], ['ms'], ['mb'], eng='pool')
        P.ld(xs[:nt, :], x[t0:t0 + nt, :], [], ['xs'])
        for n in range(4):
            for k in range(16):
                P.mm(psA[:nt, n * 512:(n + 1) * 512], mb[:, k, :nt], wb[:, k, n * 512:(n + 1) * 512], k == 0, k == 15,
                     ['mb', 'wb'], [A[n]])
        P.tt(x1[:nt, :], psA[:nt, :], bc[:nt, 0, :], ALU.mult, A + ['bc'], ['x1'])
        P.tt(x1[:nt, :], x1[:nt, :], xs[:nt, :], ALU.add, ['x1', 'xs'], ['x1'])
        P.ld(x1o[t0:t0 + nt, :], x1[:nt, :], ['x1'], ['x1o'], eng='act')
        P.act(junk[:nt, :], x1[:nt, :], AF.Square, ['x1'], ['junk', 'ssq'], accum_out=ssq[:nt, :])
        P.ts(ssq[:nt, :], ssq[:nt, :], 1.0 / 2048, 1e-6, ALU.mult, ALU.add, ['ssq'], ['ssq'])
        P.act(ssq[:nt, :], ssq[:nt, :], AF.Sqrt, ['ssq'], ['ssq'])
        P.op('dve', lambda e: e.reciprocal(rstd[:nt, :], ssq[:nt, :]), ['ssq'], ['rstd'])
        P.stt(h2[:nt, :], x1[:nt, :], rstd[:nt, 0:1], bc[:nt, 2, :], ALU.mult, ALU.mult, ['x1', 'rstd', 'bc'], ['h2'])
        P.tt(h2[:nt, :], h2[:nt, :], bc[:nt, 3, :], ALU.add, ['h2', 'bc'], ['h2'], eng='pool')
        P.ld(h2o[t0:t0 + nt, :], h2[:nt, :], ['h2'], ['h2o'], eng='act')

    t0 = 0
    for ti, nt in enumerate(tiles):
        tile_body(ti, nt, t0)
        t0 += nt
    return P


def rms(x, w):
    return x / np.sqrt((x * x).mean(-1, keepdims=True) + 1e-6) * w


def conv_tiles():
    t = [(0, 64, 0)]
    for i in range(4):
        t.append((94 + 512 * i, 512, 64 + 512 * i))
    return t


def build_conv(tiles, Lext, T):
    P = Prog()
    zx = P.inp("zx", [3072, Lext])
    dwd = P.inp("dw", [128, 8, 31])
    lnwd = P.inp("lnw", [128, 8])
    lnbd = P.inp("lnb", [128, 8])
    pwd = P.inp("pw", [128, 4, 2, 256])
    pscd = P.inp("pscale", [128, 8])
    icd = P.inp("invcnt", [4, T])
    mT = P.outp("mT", [2048, T])
    ones = P.sb("ones", [128, 128])
    dw = P.sb("dw_sb", [128, 8, 31])
    lnw = P.sb("lnw_sb", [128, 8])
    lnb = P.sb("lnb_sb", [128, 8])
    pws = P.sb("pws", [128, 4, 2, 256])
    pwb = P.sb("pwb", [128, 4, 2, 256], BF16)
    psc = P.sb("psc", [128, 8])
    ic = P.sb("ic", [128, 4, 512])
    at = [P.sb("at%d" % i, [128, 544]) for i in range(2)]
    bt = [P.sb("bt%d" % i, [128, 544]) for i in range(2)]
    ut = [P.sb("ut%d" % i, [128, 544]) for i in range(2)]
    cv = P.sb("cv", [128, 8, 512])
    sq = [P.sb("sq%d" % i, [128, 512]) for i in range(2)]
    mu = P.sb("mu", [128, 512])
    var = P.sb("var", [128, 512])
    tmp = [P.sb("tmp%d" % i, [128, 512]) for i in range(2)]
    mo = P.sb("mo", [128, 16, 512])
    pt = [P.sb("pt%d" % i, [128, 544]) for i in range(2)]
    sa = [P.sb("sa%d" % i, [128, 544]) for i in range(2)]
    sb_ = [P.sb("sb%d" % i, [128, 544]) for i in range(2)]
    plT = P.sb("plT", [128, 8, 512], BF16)
    ps1 = P.ps("ps1")
    ps2 = P.ps("ps2")
    psg = [P.ps("psg%d" % i) for i in range(2)]
    P.memset(ones[:], 1.0, ['ones'])
    P.ld(dw[:], dwd, [], ['dw'])
    P.ld(lnw[:], lnwd, [], ['lnw'])
    P.ld(lnb[:], lnbd, [], ['lnb'])
    P.ld(pws[:], pwd, [], ['pws'])
    P.cp(pwb[:], pws[:], ['pws'], ['pwb'])
    P.ld(psc[:], pscd, [], ['psc'])

    def tile_body(e0, n, o0):
        ne = n + 30
        for g in range(4):
            P.ld(ic[:, g, :n], icd[g:g + 1, o0:o0 + n].partition_broadcast(128), [], ['ic'])
        for k in range(8):
            b2 = k % 2
            P.ld(at[b2][:, :ne], zx[k * 128:(k + 1) * 128, e0:e0 + ne], [], ['at%d' % b2])
            P.ld(bt[b2][:, :ne], zx[1024 + k * 128:1024 + (k + 1) * 128, e0:e0 + ne], [], ['bt%d' % b2])
            P.act(bt[b2][:, :ne], bt[b2][:, :ne], AF.Sigmoid, ['bt%d' % b2], ['bt%d' % b2])
            P.tt(ut[b2][:, :ne], at[b2][:, :ne], bt[b2][:, :ne], ALU.mult, ['at%d' % b2, 'bt%d' % b2], ['ut%d' % b2], eng='pool')
            ck = ('cv', k)
            P.ts(cv[:, k, :n], ut[b2][:, 0:n], dw[:, k, 0:1], None, ALU.mult, None, ['ut%d' % b2, 'dw'], [ck])
            for j in range(1, 31):
                P.stt(cv[:, k, :n], ut[b2][:, j:j + n], dw[:, k, j:j + 1], cv[:, k, :n], ALU.mult, ALU.add,
                      ['ut%d' % b2, 'dw', ck], [ck])
            P.act(sq[b2][:, :n], cv[:, k, :n], AF.Square, [ck], ['sq%d' % b2])
            P.mm(ps1[:, :n], ones[:], cv[:, k, :n], k == 0, k == 7, ['ones', ck], ['ps1'])
            P.mm(ps2[:, :n], ones[:], sq[b2][:, :n], k == 0, k == 7, ['ones', 'sq%d' % b2], ['ps2'])
        P.ts(mu[:, :n], ps1[:, :n], 1.0 / 1024, None, ALU.mult, None, ['ps1'], ['mu'])
        P.tt(var[:, :n], mu[:, :n], mu[:, :n], ALU.mult, ['mu'], ['var'])
        P.stt(var[:, :n], ps2[:, :n], 1.0 / 1024, var[:, :n], ALU.mult, ALU.subtract, ['ps2', 'var'], ['var'])
        P.ts(var[:, :n], var[:, :n], 1e-6, None, ALU.add, None, ['var'], ['var'])
        P.act(var[:, :n], var[:, :n], AF.Sqrt, ['var'], ['var'])
        P.op('dve', lambda e: e.reciprocal(var[:, :n], var[:, :n]), ['var'], ['var'])
        for k in range(8):
            b2 = k % 2
            eng = 'dve' if b2 == 0 else 'pool'
            P.tt(tmp[b2][:, :n], cv[:, k, :n], mu[:, :n], ALU.subtract, [('cv', k), 'mu'], ['tmp%d' % b2], eng=eng)
            P.tt(tmp[b2][:, :n], tmp[b2][:, :n], var[:, :n], ALU.mult, ['tmp%d' % b2, 'var'], ['tmp%d' % b2], eng=eng)
            P.act(mo[:, k, :n], tmp[b2][:, :n], AF.Silu, ['tmp%d' % b2, 'lnw', 'lnb'], ['moA'],
                  bias=lnb[:, k:k + 1], scale=lnw[:, k:k + 1])
        P.ld(mT.rearrange("(k p) t -> p k t", p=128)[:, 0:8, o0:o0 + n], mo[:, 0:8, :n], ['moA'], ['mT'], eng='act')
        for gi in range(4):
            wlog = gi + 1
            wn = 2 ** wlog
            for cc in range(2):
                kk = 2 * gi + cc
                b2 = kk % 2
                r0 = 2048 + kk * 128
                P.ld(pt[b2][:, :ne], zx[r0:r0 + 128, e0:e0 + ne], [], ['pt%d' % b2])
                cur, curname, L = pt[b2], 'pt%d' % b2, ne
                step = 1
                for lv in range(wlog):
                    dst = (sa, sb_)[lv % 2][b2]
                    dname = ('sa%d', 'sb%d')[lv % 2] % b2
                    L2 = L - step
                    P.tt(dst[:, :L2], cur[:, 0:L2], cur[:, step:step + L2], ALU.add, [curname], [dname],
                         eng='pool' if b2 else 'dve')
                    cur, curname, L = dst, dname, L2
                    step *= 2
                s0 = 15 - wn // 2
                tm = tmp[b2]
                P.tt(tm[:, :n], cur[:, s0:s0 + n], ic[:, gi, :n], ALU.mult, [curname, 'ic'], ['tmp%d' % b2])
                P.tt(plT[:, kk, :n], tm[:, :n], pt[b2][:, 15:15 + n], ALU.subtract, ['tmp%d' % b2, 'pt%d' % b2], [('pl', kk)])
        for gi in range(4):
            for db in range(2):
                pb = (gi * 2 + db) % 2
                for cc in range(2):
                    P.mm(psg[pb][:, :n], pwb[:, gi, cc, db * 128:(db + 1) * 128], plT[:, 2 * gi + cc, :n], cc == 0, cc == 1,
                         ['pwb', ('pl', 2 * gi + cc)], ['psg%d' % pb])
                P.ts(mo[:, 8 + gi * 2 + db, :n], psg[pb][:, :n], psc[:, gi * 2 + db:gi * 2 + db + 1], None, ALU.mult, None,
                     ['psg%d' % pb, 'psc'], ['moB'])
        P.ld(mT.rearrange("(k p) t -> p k t", p=128)[:, 8:16, o0:o0 + n], mo[:, 8:16, :n], ['moB'], ['mT'], eng='act')

    for (e0, n, o0) in tiles:
        tile_body(e0, n, o0)
    return P


def col8(a):
    return np.ascontiguousarray(a.reshape(-1, 128).T)


def conv_host_consts(conv_dw, ln_w, ln_b, pool_w, pool_scale):
    dw = np.ascontiguousarray(conv_dw.T.reshape(8, 128, 31).transpose(1, 0, 2))
    pw = np.ascontiguousarray(pool_w.reshape(4, 2, 128, 256).transpose(2, 0, 1, 3))
    return {"dw": dw, "lnw": col8(ln_w), "lnb": col8(ln_b), "pw": pw, "pscale": col8(pool_scale)}


def invcnt_for(seg_len, t_lo, n):
    out = np.zeros((4, n), np.float32)
    t = np.arange(t_lo, t_lo + n)
    for gi, w in enumerate((2, 4, 8, 16)):
        lo = np.clip(t - w // 2, 0, seg_len)
        hi = np.clip(t + w - w // 2, 0, seg_len)
        out[gi] = 1.0 / (hi - lo).astype(np.float32)
    return out


LTOT = 8448
NCTX = 256
LLAT = 8192
CH = 32


def mix_consts():
    f = np.float64
    c = {}
    i256 = np.arange(256, dtype=f)
    a256 = 2 * np.pi * np.outer(i256, i256) / 256
    C256, S256 = np.cos(a256), np.sin(a256)
    lay = lambda m: np.ascontiguousarray(m.reshape(2, 128, 256).transpose(1, 0, 2)).astype(np.float32)
    c["c256"] = lay(C256)
    c["s256n"] = lay(-S256)
    c["cp256"] = lay(C256 / 256.0)
    c["sp256"] = lay(S256 / 256.0)
    i128 = np.arange(128, dtype=f)
    a128 = 2 * np.pi * np.outer(i128, i128) / 128
    c["c128"] = np.cos(a128).astype(np.float32)
    c["s128"] = np.sin(a128).astype(np.float32)
    c["s128n"] = (-np.sin(a128)).astype(np.float32)
    i64 = np.arange(64, dtype=f)
    atw = 2 * np.pi * np.outer(i64, i128) / 8192
    c["twc"] = np.cos(atw).astype(np.float32)
    c["tws"] = np.sin(atw).astype(np.float32)
    a64 = 2 * np.pi * np.outer(i64, i64) / 64
    sc = 1.0 / np.sqrt(8192.0 * 256.0)
    c["c64s"] = (np.cos(a64) * sc).astype(np.float32)
    c["s64s"] = (np.sin(a64) * sc).astype(np.float32)
    c["maskT"] = np.triu(np.ones((64, 64), np.float32))
    c["maskI"] = np.triu(np.ones((CH, CH), np.int32)).astype(np.int32)
    c["ident"] = np.eye(128, dtype=np.float32)
    rm = np.ones((128, 256), np.float32)
    rm[:, ::CH] = 0
    c["rmask"] = rm
    return c


def build_mix(layer_j, do_four=True, do_hg=True, nsb=33):
    P = Prog()
    NCH = 256 // CH
    aT = P.inp("aT", [256, LTOT])
    qTd = P.inp("qT", [4, 128, LTOT])
    flTd = P.inp("flT", [4, 128, LTOT])
    vvd = P.inp("vv", [4, LTOT, 128])
    lbld = P.inp("lbl", [128, 2, 2])
    cd = {}
    shapes = {"c256": [128, 2, 256], "s256n": [128, 2, 256], "cp256": [128, 2, 256], "sp256": [128, 2, 256],
              "c128": [128, 128], "s128": [128, 128], "s128n": [128, 128], "twc": [64, 128], "tws": [64, 128],
              "c64s": [64, 64], "s64s": [64, 64], "maskT": [64, 64], "ident": [128, 128], "rmask": [128, 256]}
    for k, s in shapes.items():
        cd[k] = P.inp(k, s)
    maskId = P.inp("maskI", [CH, CH], I32)
    fourT = P.outp("fourT", [256, LTOT])
    oo = P.outp("oo", [4, LTOT, 128])
    pb = [P.ps("pb%d" % i) for i in range(7)]
    pbh = P.ps("pb7", [128, 1024], BF16)
    cs = {}
    cb = {}
    for k, s in shapes.items():
        cs[k] = P.sb("cs_" + k, s)
        P.ld(cs[k][:], cd[k], [], ['cs_' + k])
        if k in ("c256", "s256n", "cp256", "sp256", "c128", "s128", "s128n", "c64s", "s64s", "ident"):
            cb[k] = P.sb("cb_" + k, s, BF16)
            P.cp(cb[k][:], cs[k][:], ['cs_' + k], ['cb_' + k])

    if do_four:
        ast = P.sb("ast", [128, 2112])
        ab = P.sb("ab", [128, 2, LTOT], BF16)
        av = aT.rearrange("(c p) t -> p c t", p=128)
        for cc in range(2):
            for q in range(4):
                P.ld(ast[:], av[:, cc, q * 2112:(q + 1) * 2112], [], ['ast'])
                P.cp(ab[:, cc, q * 2112:(q + 1) * 2112], ast[:], ['ast'], ['ab'], eng='pool' if q % 2 else 'act')
        yc = P.sb("yc", [128, 2, 2, 256], BF16)
        for blk in range(2):
            for ri, cn in enumerate(("c256", "s256n")):
                for cc in range(2):
                    P.mm(pb[ri][:, 0:256], ab[:, cc, blk * 128:(blk + 1) * 128], cb[cn][:, cc, :], cc == 0, cc == 1,
                         ['ab', 'cb_' + cn], ['pb%d' % ri])
                P.cp(yc[:, blk, ri, :], pb[ri][:, 0:256], ['pb%d' % ri], ['yc'], eng='act' if ri else 'dve')
        octx = P.sb("octx", [128, 2, 256])
        for kb in range(2):
            i = 0
            for blk in range(2):
                for ri, cn in enumerate(("cp256", "sp256")):
                    P.mm(pb[2][:, 0:256], yc[:, blk, ri, kb * 128:(kb + 1) * 128], cb[cn][:, blk, :], i == 0, i == 3,
                         ['yc', 'cb_' + cn], ['pb2'])
                    i += 1
            P.cp(octx[:, kb, :], pb[2][:, 0:256], ['pb2'], ['octx'])
        P.ld(fourT.rearrange("(k p) t -> p k t", p=128)[:, :, 0:256], octx[:], ['octx'], ['fourT'], eng='act')
        Dr = P.sb("Dr", [128, 64, 128], BF16)
        Di = P.sb("Di", [128, 64, 128], BF16)
        Br = [P.sb("Br%d" % i, [64, 4, 128], BF16) for i in range(2)]
        Bi = [P.sb("Bi%d" % i, [64, 4, 128], BF16) for i in range(2)]
        t1 = P.sb("t1", [64, 4, 128])
        t2 = P.sb("t2", [64, 4, 128])
        ostg = [P.sb("fostg%d" % i, [64, 4, 128]) for i in range(2)]
        twc4 = cs["twc"][:].unsqueeze(1).to_broadcast([64, 4, 128])
        tws4 = cs["tws"][:].unsqueeze(1).to_broadcast([64, 4, 128])
        for kh in range(2):
            for g in range(16):
                for ri, cn, Dd, dn in ((0, "c256", Dr, 'Dr'), (1, "s256n", Di, 'Di')):
                    pbi = 2 * (g % 2) + ri
                    for q in range(4):
                        nlo = g * 4 + q
                        for cc in range(2):
                            P.mm(pb[pbi][:, q * 128:(q + 1) * 128], ab[:, cc, NCTX + nlo:NCTX + LLAT:64],
                                 cb[cn][:, cc, kh * 128:(kh + 1) * 128], cc == 0, cc == 1, ['ab', 'cb_' + cn], ['pb%d' % pbi])
                    P.cp(Dd[:, g * 4:(g + 1) * 4, :].rearrange("p a b -> p (a b)"), pb[pbi][:, :], ['pb%d' % pbi], [dn],
                         eng='act' if ri else 'dve')
            for cg in range(32):
                b2 = cg % 2
                par, pai, px = pb[4], pb[5], pb[6]
                for q in range(4):
                    ch = cg * 4 + q
                    P.mm(par[0:64, q * 128:(q + 1) * 128], Dr[:, :, ch], cb["c128"][:], True, False, ['Dr', 'cb_c128'], ['pb4'])
                    P.mm(par[0:64, q * 128:(q + 1) * 128], Di[:, :, ch], cb["s128"][:], False, True, ['Di', 'cb_s128'], ['pb4'])
                    P.mm(pai[0:64, q * 128:(q + 1) * 128], Di[:, :, ch], cb["c128"][:], True, False, ['Di', 'cb_c128'], ['pb5'])
                    P.mm(pai[0:64, q * 128:(q + 1) * 128], Dr[:, :, ch], cb["s128n"][:], False, True, ['Dr', 'cb_s128n'], ['pb5'])
                ar3 = par[0:64, :].rearrange("p (a b) -> p a b", b=128)
                ai3 = pai[0:64, :].rearrange("p (a b) -> p a b", b=128)
                P.tt(t1[:], ar3, twc4, ALU.mult, ['pb4', 'cs_twc'], ['t1'])
                P.tt(t2[:], ai3, tws4, ALU.mult, ['pb5', 'cs_tws'], ['t2'])
                P.tt(Br[b2][:], t1[:], t2[:], ALU.add, ['t1', 't2'], ['Br%d' % b2], eng='pool')
                P.tt(t1[:], ai3, twc4, ALU.mult, ['pb5', 'cs_twc'], ['t1'])
                P.tt(t2[:], ar3, tws4, ALU.mult, ['pb4', 'cs_tws'], ['t2'])
                P.tt(Bi[b2][:], t1[:], t2[:], ALU.subtract, ['t1', 't2'], ['Bi%d' % b2], eng='pool')
                for q in range(4):
                    P.mm(px[0:64, q * 128:(q + 1) * 128], cb["c64s"][:], Br[b2][:, q, :], True, False, ['cb_c64s', 'Br%d' % b2], ['pb6'])
                    P.mm(px[0:64, q * 128:(q + 1) * 128], cb["s64s"][:], Bi[b2][:, q, :], False, True, ['cb_s64s', 'Bi%d' % b2], ['pb6'])
                P.cp(ostg[b2][:].rearrange("p a b -> p (a b)"), px[0:64, :], ['pb6'], ['fostg%d' % b2], eng='act')
                c0 = kh * 128 + cg * 4
                P.ld(fourT[c0:c0 + 4, NCTX:].rearrange("c (k2 k1) -> k2 c k1", k1=128), ostg[b2][:], ['fostg%d' % b2], ['fourT'],
                     eng='act')

    if do_hg:
        maskI = P.sb("maskI_sb", [CH, CH], I32)
        P.ld(maskI[:], maskId, [], ['maskI'])
        lbl = P.sb("lbl_sb", [128, 2, 2])
        lb = P.sb("lb", [128, 2])
        oml = P.sb("oml", [128, 2])
        P.ld(lbl[:], lbld, [], ['lbl'])
        if layer_j == 0:
            P.memset(lb[:], 0.0, ['lb'])
        else:
            P.tt(lb[:], lbl[:, :, 1], lbl[:, :, 0], ALU.subtract, ['lbl'], ['lb'])
            P.act(lb[:], lb[:], AF.Sigmoid, ['lb'], ['lb'])
        P.ts(oml[:], lb[:], -1.0, 1.0, ALU.mult, ALU.add, ['lb'], ['oml'])
        NS = 4
        S32 = [P.sb("S32_%d" % s, [128, 128]) for s in range(NS)]
        Sb = [P.sb("Sb_%d" % s, [128, 128], BF16) for s in range(NS)]
        for s in range(NS):
            P.memset(S32[s][:], 0.0, ['S32_%d' % s])
            P.memset(Sb[s][:], 0.0, ['Sb_%d' % s], eng='pool')

        def mk(nm, shape, dt=F32, n=2):
            return [[P.sb("%s_%d_%d" % (nm, s, i), shape, dt) for i in range(n)] for s in range(NS)]
        qs_ = mk("qs", [128, 256])
        fl_ = mk("fl", [128, 256])
        lf_ = mk("lf", [128, 256])
        kk_ = mk("kk", [128, 256])
        bb_ = mk("bb", [128, 256])
        vs_ = mk("vs", [CH, NCH, 128], n=1)
        vb_ = mk("vb", [CH, NCH, 128], BF16)
        os_ = mk("os", [CH, NCH, 128], n=1)
        nbm_ = mk("nbm", [128, NCH])
        ex = [[P.sb("ex_%d_%d" % (s, i), [128, CH]) for i in range(4)] for s in range(NS)]
        Qt = mk("Qt", [128, CH], BF16)
        Kt = mk("Kt", [128, CH], BF16)
        Qs = mk("Qs", [128, CH], BF16)
        Kp = mk("Kp", [128, CH], BF16)
        ATs = mk("ATs", [CH, CH], BF16)
        Kps = mk("Kps", [CH, 128], BF16)
        for s in range(NS):
            for i in range(2):
                P.memset(ATs[s][i][:], 0.0, ["ATs_%d_%d" % (s, i)], eng='pool')

        def nm(base, s, i):
            return "%s_%d_%d" % (base, s, i)

        for sbi in range(nsb):
            p2 = sbi % 2
            c0 = sbi * 256
            for s in range(NS):
                hh = s // 2
                P.ld(qs_[s][p2][:], qTd[s, :, c0:c0 + 256], [], [nm('qs', s, p2)])
                P.ld(fl_[s][p2][:], flTd[s, :, c0:c0 + 256], [], [nm('fl', s, p2)])
                P.ld(vs_[s][0][:], vvd[s, c0:c0 + 256, :].rearrange("(c p) d -> p c d", p=CH), [], [nm('vs', s, 0)])
                P.cp(vb_[s][p2][:], vs_[s][0][:], [nm('vs', s, 0)], [nm('vb', s, p2)], eng='pool')
                P.act(qs_[s][p2][:], qs_[s][p2][:], AF.Silu, [nm('qs', s, p2)], [nm('qs', s, p2)])
                P.act(fl_[s][p2][:], fl_[s][p2][:], AF.Sigmoid, [nm('fl', s, p2)], [nm('fl', s, p2)])
                P.ts(fl_[s][p2][:], fl_[s][p2][:], oml[:, hh:hh + 1], lb[:, hh:hh + 1], ALU.mult, ALU.add,
                     [nm('fl', s, p2), 'oml', 'lb'], [nm('fl', s, p2)])
                P.ts(kk_[s][p2][:], fl_[s][p2][:], -1.0, 1.0, ALU.mult, ALU.add, [nm('fl', s, p2)], [nm('kk', s, p2)], eng='pool')
                P.ts(fl_[s][p2][:], fl_[s][p2][:], 1e-30, None, ALU.max, None, [nm('fl', s, p2)], [nm('fl', s, p2)])
                P.act(lf_[s][p2][:], fl_[s][p2][:], AF.Ln, [nm('fl', s, p2)], [nm('lf', s, p2)])
                P.op('dve', lambda e, s=s, p2=p2: e.tensor_tensor_scan(bb_[s][p2][:], cs["rmask"][:], lf_[s][p2][:], 0.0,
                                                                       ALU.mult, ALU.add),
                     ['cs_rmask', nm('lf', s, p2)], [nm('bb', s, p2)])
                P.ts(nbm_[s][p2][:], bb_[s][p2][:, CH // 2 - 1:256:CH], -1.0, None, ALU.mult, None, [nm('bb', s, p2)], [nm('nbm', s, p2)])
            for ci in range(NCH):
                gc = sbi * NCH + ci
                c2 = gc % 2
                sl = slice(ci * CH, (ci + 1) * CH)
                for s in range(NS):
                    b_c = bb_[s][p2][:, sl]
                    bmid = bb_[s][p2][:, ci * CH + CH // 2 - 1:ci * CH + CH // 2]
                    blast = bb_[s][p2][:, ci * CH + CH - 1:ci * CH + CH]
                    nb = nbm_[s][p2][:, ci:ci + 1]
                    rb = [nm('bb', s, p2), nm('nbm', s, p2)]
                    exn = ['ex_%d_%d' % (s, i) for i in range(4)]
                    P.act(ex[s][0][:], b_c, AF.Exp, rb, [exn[0]], bias=nb, scale=1.0)
                    P.act(ex[s][1][:], b_c, AF.Exp, rb, [exn[1]], bias=bmid, scale=-1.0)
                    P.act(ex[s][2][:], b_c, AF.Exp, rb, [exn[2]])
                    P.act(ex[s][3][:], b_c, AF.Exp, rb, [exn[3]], bias=blast, scale=-1.0)
                    qn, kn = nm('qs', s, p2), nm('kk', s, p2)
                    P.tt(Qt[s][c2][:], qs_[s][p2][:, sl], ex[s][0][:], ALU.mult, [qn, exn[0]], [nm('Qt', s, c2)])
                    P.tt(Kt[s][c2][:], kk_[s][p2][:, sl], ex[s][1][:], ALU.mult, [kn, exn[1]], [nm('Kt', s, c2)], eng='pool')
                    P.tt(Qs[s][c2][:], qs_[s][p2][:, sl], ex[s][2][:], ALU.mult, [qn, exn[2]], [nm('Qs', s, c2)])
                    P.tt(Kp[s][c2][:], kk_[s][p2][:, sl], ex[s][3][:], ALU.mult, [kn, exn[3]], [nm('Kp', s, c2)], eng='pool')
                    pa = 'pb%d' % (s % 2)
                    P.mm(pb[s % 2][0:CH, 0:CH], Kt[s][c2][:], Qt[s][c2][:], True, True, [nm('Kt', s, c2), nm('Qt', s, c2)], [pa])
                    P.op('dve', lambda e, s=s, c2=c2: e.copy_predicated(ATs[s][c2][:], maskI[:], pb[s % 2][0:CH, 0:CH]),
                         [pa, 'maskI', nm('ATs', s, c2)], [nm('ATs', s, c2)])
                    P.tr(pbh[0:CH, (s % 2) * 128:(s % 2) * 128 + 128], Kp[s][c2][:], cb["ident"][:], [nm('Kp', s, c2), 'cb_ident'],
                         ['pb7_%d' % (s % 2)])
                    P.cp(Kps[s][c2][:], pbh[0:CH, (s % 2) * 128:(s % 2) * 128 + 128], ['pb7_%d' % (s % 2)], [nm('Kps', s, c2)], eng='act')
                    po = 'pb%d' % (2 + s % 2)
                    P.mm(pb[2 + s % 2][0:CH, 0:128], ATs[s][c2][:], vb_[s][p2][:, ci, :], True, False,
                         [nm('ATs', s, c2), nm('vb', s, p2)], [po])
                    P.mm(pb[2 + s % 2][0:CH, 0:128], Qs[s][c2][:], Sb[s][:], False, True, [nm('Qs', s, c2), 'Sb_%d' % s], [po])
                    P.cp(os_[s][0][:, ci, :], pb[2 + s % 2][0:CH, 0:128], [po], [nm('os', s, 0)], eng='act')
                    pS = 'pb%d' % (4 + s % 2)
                    P.mm(pb[4 + s % 2][:, 0:128], Kps[s][c2][:], vb_[s][p2][:, ci, :], True, True,
                         [nm('Kps', s, c2), nm('vb', s, p2)], [pS])
                    P.stt(S32[s][:], S32[s][:], ex[s][2][:, CH - 1:CH], pb[4 + s % 2][:, 0:128], ALU.mult, ALU.add,
                          ['S32_%d' % s, exn[2], pS], ['S32_%d' % s])
                    P.cp(Sb[s][:], S32[s][:], ['S32_%d' % s], ['Sb_%d' % s], eng='pool')
            for s in range(NS):
                P.ld(oo[s, c0:c0 + 256, :].rearrange("(c p) d -> p c d", p=CH), os_[s][0][:], [nm('os', s, 0)], ['oo'], eng='act')
    return P


def hgrn_ref(q, fl, v, lb):
    L = q.shape[0]
    sig = 1 / (1 + np.exp(-fl))
    f = lb + (1 - lb) * sig
    k = 1 - f
    qs = q / (1 + np.exp(-q))
    S = np.zeros((128, 128))
    o = np.zeros((L, 128))
    for t in range(L):
        S = f[t][:, None] * S + np.outer(k[t], v[t])
        o[t] = S.T @ qs[t]
    return o


_PROGS = {}


def _prog(key, fn):
    if key not in _PROGS:
        _PROGS[key] = fn().build()
    return _PROGS[key]


def build_add():
    P = Prog()
    xa = P.inp("xa", [2048, 2048])
    pe = P.inp("pe", [2048, 2048])
    out = P.outp("xo", [2048, 2048])
    a = [P.sb("a%d" % i, [128, 2048]) for i in range(2)]
    b = [P.sb("b%d" % i, [128, 2048]) for i in range(2)]
    for i in range(16):
        k = i % 2
        P.ld(a[k][:], xa[i * 128:(i + 1) * 128, :], [], ['a%d' % k])
        P.ld(b[k][:], pe[i * 128:(i + 1) * 128, :], [], ['b%d' % k])
        P.tt(a[k][:], a[k][:], b[k][:], ALU.add, ['a%d' % k, 'b%d' % k], ['a%d' % k], eng='dve' if k == 0 else 'pool')
        P.ld(out[i * 128:(i + 1) * 128, :], a[k][:], ['a%d' % k], ['out'], eng='act')
    return P


def _sincos_table():
    quarter = 512
    row = np.repeat(np.arange(128), 64).astype(np.float32)[:, None]
    colv = np.tile(np.arange(64), 128).astype(np.float32)[:, None]
    omega = (1.0 / (np.float32(10000.0) ** (np.arange(quarter, dtype=np.float32) / np.float32(quarter)))).astype(np.float32)
    ar, ac = (row * omega).astype(np.float32), (colv * omega).astype(np.float32)
    return np.concatenate([np.sin(ar), np.cos(ar), np.sin(ac), np.cos(ac)], axis=-1).astype(np.float32)


def _col16(a):
    return np.ascontiguousarray(np.asarray(a, np.float32).reshape(16, 128).T)


def _flipseg(a):
    return np.concatenate([a[..., :256][..., ::-1], a[..., 256:][..., ::-1]], axis=-1)


def _run(nc, maps):
    res = run_bass_kernel_spmd(nc, maps, core_ids=list(range(8)))
    return res.results


def kernel(x, c, ctx, c_ctx, ada_w, ada_b, norm1_w, norm2_w, final_norm_w, ev_in_w, ev_out_w, hg_lb_logits, hg_norm_w,
           od_in_w, od_out_w, conv_dw, conv_ln_w, conv_ln_b, pool_w, pool_scale, peer_q_w, peer_keys, peer_u, peer_v):
    f32 = lambda a: np.ascontiguousarray(np.asarray(a, dtype=np.float32))
    x, c, ctx, c_ctx = f32(x), f32(c), f32(ctx), f32(c_ctx)
    ada_w, ada_b = np.asarray(ada_w, np.float32), np.asarray(ada_b, np.float32)
    cores = [(b, s) for b in range(2) for s in range(4)]
    TT = [64, 512, 512, 512, 512]
    T128 = [64] + [128] * 16
    pe = _sincos_table()
    nc = _prog('add', build_add)
    r = _run(nc, [{"xa": f32(x[b, 2048 * s:2048 * (s + 1)]), "pe": f32(pe[2048 * s:2048 * (s + 1)])} for (b, s) in cores])
    X = [np.concatenate([ctx[b, 64 * s:64 * (s + 1)], r[i]["xo"]], axis=0) for i, (b, s) in enumerate(cores)]
    cv = np.stack([c[0], c[1], c_ctx], 0)
    cT = np.ascontiguousarray(cv.T.reshape(16, 128, 3).transpose(1, 0, 2))
    nc = _prog('mod', build_mod)
    maps = []
    for i in range(8):
        l, hf = i // 2, i % 2
        maps.append({"cT": cT, "w": f32(ada_w[l][:, hf * 6144:(hf + 1) * 6144]),
                     "b": f32(ada_b[l][None, hf * 6144:(hf + 1) * 6144])})
    r = _run(nc, maps)
    mod = [np.concatenate([r[2 * l]["mod"], r[2 * l + 1]["mod"]], axis=1).reshape(3, 6, 2048) for l in range(4)]
    mc = mix_consts()
    pc = peer_consts()
    for l in range(4):
        j = l // 2
        even = (l % 2 == 0)
        N = 6144 if even else 3072
        w_in = f32(ev_in_w[j] if even else od_in_w[j])
        w_out = f32(ev_out_w[j] if even else od_out_w[j])
        nc = _prog(('pre', N), lambda: build_pre(TT, N))
        maps = []
        for (b, s), Xc in zip(cores, X):
            mods = np.zeros((128, 2, 2, 16), np.float32)
            for rr, mv in ((0, mod[l][2]), (1, mod[l][b])):
                mods[:, rr, 0, :] = _col16(mv[0])
                mods[:, rr, 1, :] = _col16(mv[1])
            maps.append({"xT": np.ascontiguousarray(Xc.T), "w": w_in, "nw": _col16(norm1_w[l]), "mods": mods})
        r = _run(nc, maps)
        ZT = []
        for b in range(2):
            zc = [r[4 * b + s]["zT"] for s in range(4)]
            ZT.append(np.concatenate([z[:, :64] for z in zc] + [z[:, 64:] for z in zc], axis=1))
        if even:
            nc = _prog(('mix', j), lambda: build_mix(j))
            maps = []
            for (b, s) in cores:
                Z = ZT[b]
                qs, fls, vs = [], [], []
                for hh in range(2):
                    h = 2 * s + hh
                    q_ = Z[1024 + 128 * h:1024 + 128 * (h + 1)]
                    ff = Z[2048 + 128 * h:2048 + 128 * (h + 1)]
                    fb = Z[3072 + 128 * h:3072 + 128 * (h + 1)]
                    v_ = Z[4096 + 128 * h:4096 + 128 * (h + 1)]
                    qs += [q_, _flipseg(q_)]
                    fls += [ff, _flipseg(fb)]
                    vs += [v_.T, _flipseg(v_).T]
                lbl = np.zeros((128, 2, 2), np.float32)
                for hh in range(2):
                    h = 2 * s + hh
                    for rr in range(2):
                        lbl[:, hh, rr] = hg_lb_logits[rr, 128 * h:128 * (h + 1)]
                m = {"aT": f32(Z[256 * s:256 * (s + 1)]), "qT": f32(np.stack(qs, 0)), "flT": f32(np.stack(fls, 0)),
                     "vv": f32(np.stack(vs, 0)), "lbl": lbl}
                m.update(mc)
                maps.append(m)
            r = _run(nc, maps)
            mixin = []
            for (b, s) in cores:
                cols = np.r_[64 * s:64 * (s + 1), 256 + 2048 * s:256 + 2048 * (s + 1)]
                F = np.concatenate([r[4 * b + s2]["fourT"] for s2 in range(4)], axis=0)[:, cols]
                of, ob = [], []
                for h in range(8):
                    oo = r[4 * b + h // 2]["oo"]
                    hh = h % 2
                    of.append(oo[hh * 2].T[:, cols])
                    ob.append(_flipseg(oo[hh * 2 + 1].T)[:, cols])
                mixin.append({"fourT": f32(F), "ofT": f32(np.concatenate(of, 0)), "obT": f32(np.concatenate(ob, 0)),
                              "gT": f32(ZT[b][5120:6144][:, cols]), "hnw": f32(np.asarray(hg_norm_w[j]).reshape(128, 1))})
        else:
            nc = _prog('conv', lambda: build_conv(conv_tiles(), 2172, 2112))
            hc = conv_host_consts(np.asarray(conv_dw[j], np.float32), np.asarray(conv_ln_w[j], np.float32),
                                  np.asarray(conv_ln_b[j], np.float32), np.asarray(pool_w[j], np.float32),
                                  np.asarray(pool_scale[j], np.float32))
            maps = []
            for (b, s) in cores:
                Z = ZT[b]
                zc = np.pad(Z[:, :256], ((0, 0), (15, 15)))[:, 64 * s:64 * s + 94]
                zl = np.pad(Z[:, 256:], ((0, 0), (15, 15)))[:, 2048 * s:2048 * s + 2078]
                ic = np.concatenate([invcnt_for(256, 64 * s, 64), invcnt_for(8192, 2048 * s, 2048)], axis=1)
                m = {"zx": f32(np.concatenate([zc, zl], axis=1)), "invcnt": f32(ic)}
                m.update(hc)
                maps.append(m)
            r = _run(nc, maps)
            mixin = [{"mT": r[i]["mT"]} for i in range(8)]
        nc = _prog(('A', even), lambda: build_A(T128, even))
        maps = []
        for i, (b, s) in enumerate(cores):
            rows = np.zeros((2, 4, 2048), np.float32)
            for rr, mv in ((0, mod[l][2]), (1, mod[l][b])):
                rows[rr, 0] = mv[2]
                rows[rr, 1] = norm2_w[l]
                rows[rr, 2] = mv[4]
                rows[rr, 3] = mv[3]
            m = {"x": X[i], "w": w_out, "rows": rows}
            m.update(mixin[i])
            maps.append(m)
        r = _run(nc, maps)
        x1s = [r[i]["x1"] for i in range(8)]
        h2s = [r[i]["h2"] for i in range(8)]
        fin = (l == 3)
        nc = _prog(('peer', fin), lambda: build_peer(T128, final_norm=fin))
        keysT = np.ascontiguousarray(np.asarray(peer_keys[l], np.float32).reshape(16, 128, 128).transpose(2, 0, 1))
        qw, uu, vv_ = f32(peer_q_w[l]), f32(peer_u[l]), f32(peer_v[l])
        fnw = f32(np.asarray(final_norm_w).reshape(1, 2048))
        maps = []
        for i, (b, s) in enumerate(cores):
            g5 = np.stack([mod[l][2][5], mod[l][b][5]], 0).astype(np.float32)
            m = {"h2": h2s[i], "x1": x1s[i], "qw": qw, "keysT": keysT, "u": uu, "v": vv_, "g5": g5, "fnw": fnw}
            m.update(pc)
            maps.append(m)
        r = _run(nc, maps)
        X = [r[i]["x2"] for i in range(8)]
    out = np.zeros((2, 8192, 2048), np.float32)
    for i, (b, s) in enumerate(cores):
        out[b, 2048 * s:2048 * (s + 1)] = X[i][64:]
    return out
```

```python
import numpy as np
from contextlib import ExitStack
import concourse.bass as bass
import concourse.mybir as mybir
from concourse.bass_utils import run_bass_kernel_spmd

F32 = mybir.dt.float32
BF16 = mybir.dt.bfloat16
I32 = mybir.dt.int32
U32 = mybir.dt.uint32
ALU = mybir.AluOpType
AF = mybir.ActivationFunctionType
AX = mybir.AxisListType

ENGS = ('sp', 'act', 'dve', 'pe', 'pool')
SEM_LIMIT = 30000
NDMA = 8


class Prog:
    def __init__(self):
        self.nc = bass.Bass("TRN2", target_bir_lowering=False)
        self.es = ExitStack()
        self.ops = {e: [] for e in ENGS}
        self.cnt = {e: 0 for e in ENGS}
        self.last_w = {}
        self.readers = {}
        self.seen = {e: {} for e in ENGS}
        self.dma_val = {}
        self.dma_next = {e: 0 for e in ENGS}
        self.semkeys = set()
        self.n_t = 0

    def dram(self, name, shape, dtype, kind):
        return self.nc.dram_tensor(name, list(shape), dtype, kind=kind).ap()

    def inp(self, name, shape, dtype=F32):
        return self.dram(name, shape, dtype, "ExternalInput")

    def outp(self, name, shape, dtype=F32):
        return self.dram(name, shape, dtype, "ExternalOutput")

    def scratch(self, name, shape, dtype=F32):
        return self.dram(name, shape, dtype, "Internal")

    def sb(self, name, shape, dtype=F32):
        return self.es.enter_context(self.nc.sbuf_tensor(name, list(shape), dtype))

    def ps(self, name, shape=(128, 512), dtype=F32):
        return self.es.enter_context(self.nc.psum_tensor(name, list(shape), dtype))

    def _deps(self, eng, r, w):
        toks = set()
        for k in r:
            t = self.last_w.get(k)
            if t is not None:
                toks.add(t)
        for k in w:
            t = self.last_w.get(k)
            if t is not None:
                toks.add(t)
            for t in self.readers.get(k, ()):
                toks.add(t)
        waits = {}
        for (key, val) in toks:
            if eng == 'pe' and key[0] == 'pe':
                continue
            if self.seen[eng].get(key, 0) >= val:
                continue
            waits[key] = max(waits.get(key, 0), val)
        for key, val in waits.items():
            self.seen[eng][key] = val
        return list(waits.items())

    def _commit(self, tok, r, w):
        for k in r:
            self.readers.setdefault(k, []).append(tok)
        for k in w:
            self.last_w[k] = tok
            self.readers[k] = []

    def op(self, eng, fn, r=(), w=()):
        waits = self._deps(eng, r, w)
        self.cnt[eng] += 1
        c = self.cnt[eng]
        key = (eng, (c - 1) // SEM_LIMIT)
        val = (c - 1) % SEM_LIMIT + 1
        self.semkeys.add(key)
        self.ops[eng].append((waits, fn, key, 1))
        self._commit((key, val), r, w)

    def dma(self, eng, fn, r=(), w=()):
        i = self.dma_next[eng]
        self.dma_next[eng] = (i + 1) % NDMA
        key = ('d' + eng, i)
        self.semkeys.add(key)
        prev = self.dma_val.get(key, 0)
        waits = self._deps(eng, r, w)
        if prev > 0 and self.seen[eng].get(key, 0) < prev:
            waits.append((key, prev))
            self.seen[eng][key] = prev
        val = prev + 16
        self.dma_val[key] = val
        self.ops[eng].append((waits, fn, key, 16))
        self._commit((key, val), r, w)

    def mm(self, out, lhsT, rhs, start, stop, r, w):
        self.op('pe', lambda e: e.matmul(out, lhsT, rhs, start=start, stop=stop), r, w)

    def tr(self, out, in_, ident, r, w):
        self.op('pe', lambda e: e.transpose(out, in_, ident), r, w)

    def act(self, out, in_, func, r, w, bias=None, scale=None, accum_out=None):
        kw = {}
        if bias is not None:
            kw['bias'] = bias
        if scale is not None:
            kw['scale'] = scale
        if accum_out is not None:
            kw['accum_out'] = accum_out
        self.op('act', lambda e: e.activation(out, in_, func, **kw), r, w)

    def tt(self, out, in0, in1, op, r, w, eng='dve'):
        self.op(eng, lambda e: e.tensor_tensor(out, in0, in1, op), r, w)

    def ts(self, out, in0, s1, s2, op0, op1, r, w, eng='dve', accum_out=None):
        if accum_out is not None:
            self.op(eng, lambda e: e.tensor_scalar(out, in0, s1, s2, op0, op1, accum_out), r, w)
        elif op1 is None:
            self.op(eng, lambda e: e.tensor_scalar(out, in0, s1, None, op0), r, w)
        else:
            self.op(eng, lambda e: e.tensor_scalar(out, in0, s1, s2, op0, op1), r, w)

    def stt(self, out, in0, scalar, in1, op0, op1, r, w, eng='dve'):
        self.op(eng, lambda e: e.scalar_tensor_tensor(out, in0, scalar, in1, op0, op1), r, w)

    def cp(self, out, in_, r, w, eng='dve'):
        if eng == 'act':
            self.op(eng, lambda e: e.copy(out, in_), r, w)
        else:
            self.op(eng, lambda e: e.tensor_copy(out, in_), r, w)

    def memset(self, ap, val, w, eng='dve'):
        self.op(eng, lambda e: e.memset(ap, val), (), w)

    def ld(self, out, in_, r, w, eng='sp'):
        self.dma(eng, lambda e: e.dma_start(out=out, in_=in_), r, w)

    def build(self):
        nc = self.nc
        fin = []
        for key, val in self.dma_val.items():
            if self.seen['sp'].get(key, 0) < val:
                fin.append((key, val))
        for e in ENGS:
            c = self.cnt[e]
            if c > 0:
                key = (e, (c - 1) // SEM_LIMIT)
                val = (c - 1) % SEM_LIMIT + 1
                if e != 'sp' and self.seen['sp'].get(key, 0) < val:
                    fin.append((key, val))
        sems = {}
        for key in sorted(self.semkeys, key=str):
            nm = "s_%s_%s" % (key[0], key[1])
            sems[key] = self.es.enter_context(nc.semaphore(nm))
        ops = self.ops

        def replay(ename, eng):
            for (waits, fn, key, inc) in ops[ename]:
                for (k, v) in waits:
                    eng.wait_ge(sems[k], v)
                ins = fn(eng)
                ins.then_inc(sems[key], inc)

        with nc.Block() as block:
            @block.sync
            def _(e):
                replay('sp', e)
                for (k, v) in fin:
                    e.wait_ge(sems[k], v)

            @block.scalar
            def _(e):
                replay('act', e)

            @block.vector
            def _(e):
                replay('dve', e)

            @block.tensor
            def _(e):
                replay('pe', e)

            @block.gpsimd
            def _(e):
                replay('pool', e)
        self.es.close()
        return nc

    def n_ins(self):
        return {e: len(self.ops[e]) for e in ENGS}


def build_mod():
    P = Prog()
    cT = P.inp("cT", [128, 16, 3])
    w = P.inp("w", [2048, 6144])
    b = P.inp("b", [1, 6144])
    out = P.outp("mod", [3, 6144])
    c_sb = P.sb("c_sb", [128, 16, 3])
    sc = P.sb("sc", [128, 16, 3])
    b_sb = P.sb("b_sb", [1, 6144])
    ones = P.sb("ones", [1, 4])
    o_sb = P.sb("o_sb", [3, 6144])
    wb = [P.sb("wb%d" % i, [128, 16, 512]) for i in range(2)]
    pss = [P.ps("ps%d" % i) for i in range(2)]
    P.ld(c_sb[:], cT, [], ['c_sb'])
    P.ld(b_sb[:], b, [], ['b_sb'])
    P.memset(ones[:], 1.0, ['ones'])
    P.act(sc[:], c_sb[:], AF.Silu, ['c_sb'], ['sc'])
    wv = w.rearrange("(k p) n -> p k n", p=128)
    for nt in range(12):
        wbuf = wb[nt % 2]
        ps = pss[nt % 2]
        P.ld(wbuf[:], wv[:, :, nt * 512:(nt + 1) * 512], [], ['wb%d' % (nt % 2)])
        for k in range(16):
            P.mm(ps[0:3, :], sc[:, k, :], wbuf[:, k, :], k == 0, False, ['sc', 'wb%d' % (nt % 2)], ['ps%d' % (nt % 2)])
        P.mm(ps[0:3, :], ones[0:1, 0:3], b_sb[0:1, nt * 512:(nt + 1) * 512], False, True, ['ones', 'b_sb'], ['ps%d' % (nt % 2)])
        P.cp(o_sb[:, nt * 512:(nt + 1) * 512], ps[0:3, :], ['ps%d' % (nt % 2)], ['o_sb'])
    P.ld(out, o_sb[:], ['o_sb'], ['out'])
    return P


NEG = -1.0e30


def build_peer(tiles, final_norm=False, nug=4, bft=True):
    T = sum(tiles)
    P = Prog()
    h2 = P.inp("h2", [T, 2048])
    x1 = P.inp("x1", [T, 2048])
    qw = P.inp("qw", [2048, 2048])
    keysT = P.inp("keysT", [128, 16, 128])
    u = P.inp("u", [16384, 2048])
    v = P.inp("v", [16384, 2048])
    g5 = P.inp("g5", [2, 2048])
    fnw = P.inp("fnw", [1, 2048])
    identd = P.inp("ident", [128, 128])
    iotad = P.inp("iota16", [128, 16])
    out = P.outp("x2", [T, 2048])

    ident = P.sb("ident_sb", [128, 128])
    identb = P.sb("identb", [128, 128], BF16)
    iota = P.sb("iota_sb", [128, 16])
    colsel = P.sb("colsel", [128, 256], BF16)
    qwb = P.sb("qwb", [128, 16, 2048], BF16)
    keysb = P.sb("keysb", [128, 16, 128], BF16)
    g5bc = P.sb("g5bc", [128, 2048])
    fnbc = P.sb("fnbc", [128, 2048])
    h2s = P.sb("h2s", [128, 2048])
    h2b = P.sb("h2b", [128, 2048], BF16)
    hT = P.sb("hT", [128, 16, 128], BF16)
    qT = P.sb("qT", [128, 16, 128], BF16)
    s_sb = P.sb("s_sb", [128, 2048])
    sw = P.sb("sw", [128, 256])
    tops = P.sb("tops", [128, 16, 16])
    topi = P.sb("topi", [128, 16, 16], U32)
    topif = P.sb("topif", [128, 16, 16])
    cand = P.sb("cand", [128, 8, 256])
    best = P.sb("best", [128, 8, 16])
    bpos = P.sb("bpos", [128, 8, 16], U32)
    hi_u = P.sb("hi_u", [128, 128], U32)
    lo_u = P.sb("lo_u", [128, 128], U32)
    hif = P.sb("hif", [128, 8, 16])
    lof = P.sb("lof", [128, 8, 16])
    oh = P.sb("oh", [128, 16, 16])
    sel0 = P.sb("sel0", [128, 8, 16])
    sel1 = P.sb("sel1", [128, 8, 16])
    idxf = P.sb("idxf", [128, 128])
    gate = P.sb("gate", [128, 8, 16])
    gsum = P.sb("gsum", [128, 8])
    idxT2 = [P.sb("idxT%d" % i, [128, 128], I32) for i in range(2)]
    idx_tm = P.sb("idx_tm", [128, 128], I32)
    act_tm = P.sb("act_tm", [128, 128])
    coef_tm = P.sb("coef_tm", [128, 128])
    coefT2 = [P.sb("coefT%d" % i, [128, 128]) for i in range(2)]
    junk = P.sb("junk", [128, 2048], BF16)
    NG = 2 * nug
    G = [P.sb("G%d" % i, [128, 2048], BF16) for i in range(NG)]
    Z = [P.sb("Z%d" % i, [128, 128], BF16) for i in range(2)]
    xs = P.sb("xs", [128, 2048])
    xo = P.sb("xo", [128, 2048])
    ssq = P.sb("ssq", [128, 1])
    rstd = P.sb("rstd", [128, 1])
    psA = P.ps("psA", [128, 2048])
    psB = P.ps("psB", [128, 2048])
    A = ['psA0', 'psA1', 'psA2', 'psA3']

    P.ld(ident[:], identd, [], ['ident'])
    P.ld(iota[:], iotad, [], ['iota'])
    P.cp(identb[:], ident[:], ['ident'], ['identb'])
    P.memset(colsel[:], 0.0, ['colsel'])
    P.memset(colsel[:, 128:129], 1.0, ['colsel'])
    qv = qw.rearrange("(k p) n -> p k n", p=128)
    for k in range(16):
        P.ld(qwb[:, k, :], qv[:, k, :], [], [('qwb', k)], eng='pool')
    P.ld(keysb[:].rearrange("p j k -> p (j k)"), keysT.rearrange("p j k -> p (j k)"), [], ['keysb'], eng='pool')
    P.ld(fnbc[:], fnw.partition_broadcast(128), [], ['fnbc'])

    if bft:
        ubf = P.scratch("ubf", [16384, 2048], BF16)
        vbf = P.scratch("vbf", [16384, 2048], BF16)
        cb_ = G
        cn_ = ['G%d' % i for i in range(NG)]
        ci_ = 0
        for (src_, dst_, tag_) in ((u, ubf, 'ubf'), (v, vbf, 'vbf')):
            for ch in range(128):
                b_ = ci_ % len(cb_)
                ci_ += 1
                P.ld(cb_[b_][:], src_[ch * 128:(ch + 1) * 128, :], [], [cn_[b_]], eng='pool')
                P.ld(dst_[ch * 128:(ch + 1) * 128, :], cb_[b_][:], [cn_[b_]], [(tag_, ch)], eng='sp' if ch % 2 else 'act')
        usrc, vsrc = ubf, vbf
        udep = [('ubf', ch) for ch in range(128)]
        vdep = [('vbf', ch) for ch in range(128)]
    else:
        usrc, vsrc, udep, vdep = u, v, [], []

    B = ['psB0', 'psB1', 'psB2', 'psB3']
    gring = [0]

    def front(ti, nt, t0):
        ib = ti % 2
        P.ld(h2s[:nt, :], h2[t0:t0 + nt, :], [], ['h2s'])
        P.cp(h2b[:nt, :], h2s[:nt, :], ['h2s'], ['h2b'], eng='pool')
        for k in range(16):
            bk = k % 4
            P.tr(psB[:, bk * 512:bk * 512 + nt], h2s[:nt, k * 128:(k + 1) * 128], ident[:nt, :nt], ['h2s', 'ident'], [B[bk]])
            P.op('act', lambda e, k=k, bk=bk: e.copy(hT[:, k, :nt], psB[:, bk * 512:bk * 512 + nt]), [B[bk]], ['hT'])
            if k % 4 == 3:
                yield
        for j in range(16):
            bk = j % 4
            for k in range(16):
                P.mm(psB[:, bk * 512:bk * 512 + nt], qwb[:, k, j * 128:(j + 1) * 128], hT[:, k, :nt], k == 0, k == 15,
                     [('qwb', k), 'hT'], [B[bk]])
            P.op('act', lambda e, j=j, bk=bk: e.copy(qT[:, j, :nt], psB[:, bk * 512:bk * 512 + nt]), [B[bk]], ['qT'])
            yield
        for j in range(16):
            bk = j // 4
            P.mm(psB[:nt, j * 128:(j + 1) * 128], qT[:, j, :nt], keysb[:, j, :], True, True, ['qT', 'keysb'], [B[bk]])
        for bk in range(4):
            P.op('act', lambda e, bk=bk: e.copy(s_sb[:nt, bk * 512:(bk + 1) * 512], psB[:nt, bk * 512:(bk + 1) * 512]),
                 [B[bk]], ['s_sb'])
        yield
        for j in range(16):
            sj = s_sb[:nt, j * 128:(j + 1) * 128]
            P.op('dve', lambda e, j=j, sj=sj: e.max(tops[:nt, j, 0:8], sj), ['s_sb'], ['tops'])
            P.op('dve', lambda e, j=j, sj=sj: e.max_index(topi[:nt, j, 0:8], tops[:nt, j, 0:8], sj), ['s_sb', 'tops'], ['topi'])
            P.op('dve', lambda e, j=j, sj=sj: e.match_replace(sw[:nt, 0:128], tops[:nt, j, 0:8], sj, NEG), ['s_sb', 'tops'], ['sw'])
            P.op('dve', lambda e, j=j: e.max(tops[:nt, j, 8:16], sw[:nt, 0:128]), ['sw'], ['tops'])
            P.op('dve', lambda e, j=j: e.max_index(topi[:nt, j, 8:16], tops[:nt, j, 8:16], sw[:nt, 0:128]), ['sw', 'tops'], ['topi'])
            yield
        P.cp(topif[:nt], topi[:nt], ['topi'], ['topif'])
        for h in range(8):
            c3 = cand[:nt, h, :].rearrange("t (a b) -> t a b", b=16)
            a0 = tops[:nt, 2 * h, :].unsqueeze(2).to_broadcast([nt, 16, 16])
            a1 = tops[:nt, 2 * h + 1, :].unsqueeze(1).to_broadcast([nt, 16, 16])
            P.tt(c3, a0, a1, ALU.add, ['tops'], ['cand'])
            cs = cand[:nt, h, :]
            P.op('dve', lambda e, h=h, cs=cs: e.max(best[:nt, h, 0:8], cs), ['cand'], ['best'])
            P.op('dve', lambda e, h=h, cs=cs: e.max_index(bpos[:nt, h, 0:8], best[:nt, h, 0:8], cs), ['cand', 'best'], ['bpos'])
            P.op('dve', lambda e, h=h, cs=cs: e.match_replace(sw[:nt, :], best[:nt, h, 0:8], cs, NEG), ['cand', 'best'], ['sw'])
            P.op('dve', lambda e, h=h: e.max(best[:nt, h, 8:16], sw[:nt, :]), ['sw'], ['best'])
            P.op('dve', lambda e, h=h: e.max_index(bpos[:nt, h, 8:16], best[:nt, h, 8:16], sw[:nt, :]), ['sw', 'best'], ['bpos'])
            yield
        bp2 = bpos[:nt].rearrange("t h r -> t (h r)")
        P.op('dve', lambda e: e.tensor_single_scalar(hi_u[:nt, :], bp2, 4, ALU.logical_shift_right), ['bpos'], ['hi_u'])
        P.op('dve', lambda e: e.tensor_single_scalar(lo_u[:nt, :], bp2, 15, ALU.bitwise_and), ['bpos'], ['lo_u'])
        P.cp(hif[:nt].rearrange("t h r -> t (h r)"), hi_u[:nt, :], ['hi_u'], ['hif'])
        P.cp(lof[:nt].rearrange("t h r -> t (h r)"), lo_u[:nt, :], ['lo_u'], ['lof'])
        io3 = iota[:nt, :].unsqueeze(1).to_broadcast([nt, 16, 16])
        for h in range(8):
            for (srcf, jj, dst, dn) in ((hif, 2 * h, sel0, 'sel0'), (lof, 2 * h + 1, sel1, 'sel1')):
                P.tt(oh[:nt], io3, srcf[:nt, h, :].unsqueeze(2).to_broadcast([nt, 16, 16]), ALU.is_equal, ['iota', 'hif', 'lof'], ['oh'])
                P.tt(oh[:nt], oh[:nt], topif[:nt, jj, :].unsqueeze(1).to_broadcast([nt, 16, 16]), ALU.mult, ['oh', 'topif'], ['oh'])
                P.op('dve', lambda e, dst=dst, h=h: e.reduce_sum(dst[:nt, h, :], oh[:nt], AX.X), ['oh'], [dn])
            yield
        P.stt(idxf[:nt, :], sel0[:nt].rearrange("t h r -> t (h r)"), 128.0, sel1[:nt].rearrange("t h r -> t (h r)"),
              ALU.mult, ALU.add, ['sel0', 'sel1'], ['idxf'])
        P.cp(idx_tm[:nt, :], idxf[:nt, :], ['idxf'], ['idx_tm'])
        P.tr(psB[:, 0:nt], idxf[:nt, :], ident[:nt, :nt], ['idxf', 'ident'], [B[0]])
        P.cp(idxT2[ib][:, :nt], psB[:, 0:nt], [B[0]], ['idxT%d' % ib])
        P.tt(gate[:nt], best[:nt], best[:nt, :, 0:1].to_broadcast([nt, 8, 16]), ALU.subtract, ['best'], ['gate'])
        P.act(gate[:nt], gate[:nt], AF.Exp, ['gate'], ['gate'])
        P.op('dve', lambda e: e.reduce_sum(gsum[:nt, :], gate[:nt], AX.X), ['gate'], ['gsum'])
        P.op('dve', lambda e: e.reciprocal(gsum[:nt, :], gsum[:nt, :]), ['gsum'], ['gsum'])
        P.tt(gate[:nt], gate[:nt], gsum[:nt, :].unsqueeze(2).to_broadcast([nt, 8, 16]), ALU.mult, ['gate', 'gsum'], ['gate'])
        yield

    def p1_step(ti, nt, j):
        b = gring[0] % NG
        gring[0] += 1
        P.dma('pool', lambda e, b=b, j=j: e.indirect_dma_start(
            out=G[b][:nt, :], out_offset=None, in_=usrc,
            in_offset=bass.IndirectOffsetOnAxis(ap=idx_tm[:nt, j:j + 1], axis=0)), ['idx_tm'] + udep, ['G%d' % b])
        P.op('dve', lambda e, b=b, j=j: e.scalar_tensor_tensor(
            junk[:nt, :], G[b][:nt, :], 1.0, h2b[:nt, :], ALU.mult, ALU.mult, act_tm[:nt, j:j + 1]),
            ['G%d' % b, 'h2b'], ['junk', 'act_tm'])

    def p1_fin(ti, nt):
        ib = ti % 2
        P.act(coef_tm[:nt, :], act_tm[:nt, :], AF.Gelu, ['act_tm'], ['coef_tm'])
        P.tt(coef_tm[:nt, :], coef_tm[:nt, :], gate[:nt].rearrange("t h r -> t (h r)"), ALU.mult, ['coef_tm', 'gate'], ['coef_tm'])
        P.tr(psB[:, 512:512 + nt], coef_tm[:nt, :], ident[:nt, :nt], ['coef_tm', 'ident'], [B[1]])
        P.cp(coefT2[ib][:, :nt], psB[:, 512:512 + nt], [B[1]], ['coefT%d' % ib])

    def p2_begin(ti, nt, t0):
        if ti <= 1:
            row = 0 if ti == 0 else 1
            P.ld(g5bc[:], g5[row:row + 1, :].partition_broadcast(128), [], ['g5bc'])
        P.ld(xs[:nt, :], x1[t0:t0 + nt, :], [], ['xs'])

    def p2_step(ti, nt, t):
        ib = ti % 2
        b = gring[0] % NG
        gring[0] += 1
        zb = t % 2
        P.dma('pool', lambda e, b=b, t=t: e.indirect_dma_start(
            out=G[b][:], out_offset=None, in_=vsrc,
            in_offset=bass.IndirectOffsetOnAxis(ap=idxT2[ib][:, t:t + 1], axis=0)), ['idxT%d' % ib] + vdep, ['G%d' % b])
        P.op('act', lambda e, zb=zb, t=t: e.activation(Z[zb][:, :], colsel[:, 128 - t:256 - t], AF.Identity,
                                                       scale=coefT2[ib][:, t:t + 1]),
             ['colsel', 'coefT%d' % ib], ['Z%d' % zb])
        for n in range(4):
            P.mm(psA[:nt, n * 512:(n + 1) * 512], Z[zb][:, :nt], G[b][:, n * 512:(n + 1) * 512], t == 0, t == nt - 1,
                 ['Z%d' % zb, 'G%d' % b], [A[n]])

    def p2_fin(ti, nt, t0):
        P.tt(xo[:nt, :], psA[:nt, :], g5bc[:nt, :], ALU.mult, A + ['g5bc'], ['xo'])
        P.tt(xo[:nt, :], xo[:nt, :], xs[:nt, :], ALU.add, ['xo', 'xs'], ['xo'])
        if final_norm:
            P.act(s_sb[:nt, :], xo[:nt, :], AF.Square, ['xo'], ['s_sb', 'ssq'], accum_out=ssq[:nt, :])
            P.ts(ssq[:nt, :], ssq[:nt, :], 1.0 / 2048, 1e-6, ALU.mult, ALU.add, ['ssq'], ['ssq'])
            P.act(ssq[:nt, :], ssq[:nt, :], AF.Sqrt, ['ssq'], ['ssq'])
            P.op('dve', lambda e: e.reciprocal(rstd[:nt, :], ssq[:nt, :]), ['ssq'], ['rstd'])
            P.stt(xo[:nt, :], xo[:nt, :], rstd[:nt, 0:1], fnbc[:nt, :], ALU.mult, ALU.mult, ['xo', 'rstd', 'fnbc'], ['xo'])
        P.ld(out[t0:t0 + nt, :], xo[:nt, :], ['xo'], ['out'])

    starts = [sum(tiles[:i]) for i in range(len(tiles))]
    n_t = len(tiles)
    for ti in range(n_t):
        nt, t0 = tiles[ti], starts[ti]
        pv = None
        if ti > 0:
            pnt, pt0 = tiles[ti - 1], starts[ti - 1]
            p2_begin(ti - 1, pnt, pt0)
            pv = iter(range(pnt))
        vleft = tiles[ti - 1] if ti > 0 else 0

        def vstep():
            t = next(pv, None) if pv is not None else None
            if t is not None:
                p2_step(ti - 1, pnt, t)
                return 1
            return 0
        for _ in front(ti, nt, t0):
            vleft -= vstep()
        rem = max(vleft, 0)
        done = 0
        for j in range(128):
            p1_step(ti, nt, j)
            want = (rem * (j + 1)) // 128
            while done < want:
                done += vstep()
        if ti > 0:
            while vstep():
                pass
            p2_fin(ti - 1, pnt, pt0)
        p1_fin(ti, nt)
    lt = n_t - 1
    p2_begin(lt, tiles[lt], starts[lt])
    for t in range(tiles[lt]):
        p2_step(lt, tiles[lt], t)
    p2_fin(lt, tiles[lt], starts[lt])
    return P


def peer_consts():
    return {"ident": np.eye(128, dtype=np.float32),
            "iota16": np.tile(np.arange(16, dtype=np.float32)[None, :], (128, 1))}


def peer_ref(h2, x1, qw, keys, u, v, g5rows, fnw, final_norm):
    T = h2.shape[0]
    q = (h2 @ qw).reshape(T, 8, 2, 128)
    s = np.einsum('thpd,hpkd->thpk', q, keys)
    ti = np.argsort(-s, axis=-1)[..., :16]
    ts_ = np.take_along_axis(s, ti, -1)
    cs = ts_[:, :, 0, :, None] + ts_[:, :, 1, None, :]
    ci = ti[:, :, 0, :, None] * 128 + ti[:, :, 1, None, :]
    cs = cs.reshape(T, 8, 256)
    ci = ci.reshape(T, 8, 256)
    bj = np.argsort(-cs, axis=-1)[..., :16]
    bs = np.take_along_axis(cs, bj, -1)
    idx = np.take_along_axis(ci, bj, -1)
    g = np.exp(bs - bs.max(-1, keepdims=True))
    g = g / g.sum(-1, keepdims=True)
    idx = idx.reshape(T, 128)
    g = g.reshape(T, 128)
    from scipy.special import erf
    y = np.zeros((T, 2048), np.float32)
    for t in range(T):
        a = u[idx[t]] @ h2[t]
        a = 0.5 * a * (1 + erf(a / np.sqrt(2)))
        y[t] = (g[t] * a) @ v[idx[t]]
    xo = x1 + g5rows * y
    if final_norm:
        xo = xo / np.sqrt((xo * xo).mean(-1, keepdims=True) + 1e-6) * fnw
    return xo, idx, g


def build_pre(ttiles, N):
    T = sum(ttiles)
    P = Prog()
    xT = P.inp("xT", [2048, T])
    w = P.inp("w", [2048, N])
    nwd = P.inp("nw", [128, 16])
    modd = P.inp("mods", [128, 2, 2, 16])
    zT = P.outp("zT", [N, T])
    ones = P.sb("ones", [128, 128])
    nw = P.sb("nw_sb", [128, 16])
    mods = P.sb("mods_sb", [128, 2, 2, 16])
    gg = P.sb("gg", [128, 2, 16])
    xt = P.sb("xt", [128, 16, 512])
    sq = [P.sb("sq%d" % i, [128, 512]) for i in range(2)]
    rstd = P.sb("rstd", [128, 512])
    tmp = [P.sb("tmp%d" % i, [128, 512]) for i in range(2)]
    hT = P.sb("hT", [128, 16, T], BF16)
    CG = 256
    wst = [P.sb("wst%d" % i, [128, 16, CG]) for i in range(2)]
    wbf = [P.sb("wbf%d" % i, [128, 16, CG], BF16) for i in range(2)]
    ostg = [P.sb("ostg%d" % i, [128, T]) for i in range(2)]
    pss = P.ps("pss")
    psm = [P.ps("psm%d" % i) for i in range(4)]
    P.memset(ones[:], 1.0, ['ones'])
    P.ld(nw[:], nwd, [], ['nw'])
    P.ld(mods[:], modd, [], ['mods'])
    for r in range(2):
        P.ts(gg[:, r, :], mods[:, r, 1, :], 1.0, None, ALU.add, None, ['mods'], ['gg'])
        P.tt(gg[:, r, :], gg[:, r, :], nw[:], ALU.mult, ['gg', 'nw'], ['gg'])
    xv = xT.rearrange("(k p) t -> p k t", p=128)

    def tile_body(ti, nt, t0):
        r = 0 if ti == 0 else 1
        P.ld(xt[:, :, :nt], xv[:, :, t0:t0 + nt], [], ['xt'])
        for k in range(16):
            s = sq[k % 2]
            P.act(s[:, :nt], xt[:, k, :nt], AF.Square, ['xt'], ['sq%d' % (k % 2)])
            P.mm(pss[:, :nt], ones[:], s[:, :nt], k == 0, k == 15, ['ones', 'sq%d' % (k % 2)], ['pss'])
        P.ts(rstd[:, :nt], pss[:, :nt], 1.0 / 2048, 1e-6, ALU.mult, ALU.add, ['pss'], ['rstd'])
        P.act(rstd[:, :nt], rstd[:, :nt], AF.Sqrt, ['rstd'], ['rstd'])
        P.op('dve', lambda e: e.reciprocal(rstd[:, :nt], rstd[:, :nt]), ['rstd'], ['rstd'])
        for k in range(16):
            tm = tmp[k % 2]
            eng = 'dve' if k % 2 == 0 else 'pool'
            P.tt(tm[:, :nt], xt[:, k, :nt], rstd[:, :nt], ALU.mult, ['xt', 'rstd'], ['tmp%d' % (k % 2)], eng=eng)
            P.ts(hT[:, k, t0:t0 + nt], tm[:, :nt], gg[:, r, k:k + 1], mods[:, r, 0, k:k + 1], ALU.mult, ALU.add,
                 ['tmp%d' % (k % 2), 'gg', 'mods'], [('hT', ti)], eng=eng)

    t0 = 0
    for ti, nt in enumerate(ttiles):
        tile_body(ti, nt, t0)
        t0 += nt
    hall = [('hT', ti) for ti in range(len(ttiles))]
    wv = w.rearrange("(k p) n -> p k n", p=128)
    ncg = N // CG
    cnt = 0
    for cg in range(ncg):
        wb = cg % 2
        P.ld(wst[wb][:], wv[:, :, cg * CG:(cg + 1) * CG], [], ['wst%d' % wb])
        P.cp(wbf[wb][:, 0:8, :], wst[wb][:, 0:8, :], ['wst%d' % wb], ['wbf%d' % wb], eng='pool')
        P.cp(wbf[wb][:, 8:16, :], wst[wb][:, 8:16, :], ['wst%d' % wb], ['wbf%d' % wb], eng='act')
        for cb in range(CG // 128):
            ob = (cg * (CG // 128) + cb) % 2
            t0 = 0
            for ti, nt in enumerate(ttiles):
                pb = cnt % 4
                cnt += 1
                for k in range(16):
                    P.mm(psm[pb][:, :nt], wbf[wb][:, k, cb * 128:(cb + 1) * 128], hT[:, k, t0:t0 + nt], k == 0, k == 15,
                         ['wbf%d' % wb, ('hT', ti)], ['psm%d' % pb])
                if cnt % 2 == 0:
                    P.cp(ostg[ob][:, t0:t0 + nt], psm[pb][:, :nt], ['psm%d' % pb], ['ostg%d' % ob])
                else:
                    P.op('act', lambda e, ob=ob, pb=pb, t0=t0, nt=nt: e.copy(ostg[ob][:, t0:t0 + nt], psm[pb][:, :nt]),
                         ['psm%d' % pb], ['ostg%d' % ob])
                t0 += nt
            c0 = cg * CG + cb * 128
            P.ld(zT[c0:c0 + 128, :], ostg[ob][:], ['ostg%d' % ob], ['zT'], eng='act')
    return P


def build_A(tiles, even):
    T = sum(tiles)
    P = Prog()
    if even:
        fourT = P.inp("fourT", [1024, T])
        ofT = P.inp("ofT", [1024, T])
        obT = P.inp("obT", [1024, T])
        gT = P.inp("gT", [1024, T])
        hnwd = P.inp("hnw", [128, 1])
    else:
        mTd = P.inp("mT", [2048, T])
    x = P.inp("x", [T, 2048])
    w = P.inp("w", [2048, 2048])
    rows = P.inp("rows", [2, 4, 2048])
    x1o = P.outp("x1", [T, 2048])
    h2o = P.outp("h2", [T, 2048])
    wb = P.sb("wb", [128, 16, 2048], BF16)
    stg = [P.sb("stg%d" % i, [128, 2048]) for i in range(2)]
    bc = P.sb("bc", [128, 4, 2048])
    ms = P.sb("ms", [128, 16, 128])
    mb = P.sb("mb", [128, 16, 128], BF16)
    xs = P.sb("xs", [128, 2048])
    x1 = P.sb("x1s", [128, 2048])
    h2 = P.sb("h2s", [128, 2048])
    junk = P.sb("junk", [128, 2048])
    ssq = P.sb("ssq", [128, 1])
    rstd = P.sb("rstd", [128, 1])
    psA = P.ps("psA", [128, 2048])
    A = ['psA0', 'psA1', 'psA2', 'psA3']
    if even:
        ones = P.sb("ones", [128, 128])
        hnw = P.sb("hnw_sb", [128, 1])
        o1 = P.sb("o1", [128, 8, 128])
        o2 = P.sb("o2", [128, 8, 128])
        gs = P.sb("gs", [128, 8, 128])
        osq = P.sb("osq", [128, 8, 128])
        orst = P.sb("orst", [128, 8, 128])
        psO = P.ps("psO", [128, 1024])
        P.memset(ones[:], 1.0, ['ones'])
        P.ld(hnw[:], hnwd, [], ['hnw'])
    wv = w.rearrange("(k p) n -> p k n", p=128)
    for k in range(16):
        P.ld(stg[k % 2][:], wv[:, k, :], [], ['stg%d' % (k % 2)])
        P.cp(wb[:, k, :], stg[k % 2][:], ['stg%d' % (k % 2)], ['wb'], eng='pool' if k % 2 else 'act')

    def tile_body(ti, nt, t0):
        if ti <= 1:
            for q in range(4):
                P.ld(bc[:, q, :], rows[ti, q:q + 1, :].partition_broadcast(128), [], ['bc'])
            P.ts(bc[:, 2, :], bc[:, 2, :], 1.0, None, ALU.add, None, ['bc'], ['bc'])
            P.tt(bc[:, 2, :], bc[:, 2, :], bc[:, 1, :], ALU.mult, ['bc'], ['bc'])
        if even:
            P.ld(ms[:, 0:8, :nt], fourT.rearrange("(k p) t -> p k t", p=128)[:, :, t0:t0 + nt], [], ['ms'])
            P.ld(o1[:, :, :nt], ofT.rearrange("(k p) t -> p k t", p=128)[:, :, t0:t0 + nt], [], ['o1'])
            P.ld(o2[:, :, :nt], obT.rearrange("(k p) t -> p k t", p=128)[:, :, t0:t0 + nt], [], ['o2'])
            P.ld(gs[:, :, :nt], gT.rearrange("(k p) t -> p k t", p=128)[:, :, t0:t0 + nt], [], ['gs'])
            P.tt(o1[:, :, :nt], o1[:, :, :nt], o2[:, :, :nt], ALU.add, ['o1', 'o2'], ['o1'])
            P.act(osq[:, :, :nt], o1[:, :, :nt], AF.Square, ['o1'], ['osq'])
            for hh in range(8):
                P.mm(psO[:, hh * 128:hh * 128 + nt], ones[:], osq[:, hh, :nt], True, True, ['ones', 'osq'], ['psO'])
            for hh in range(8):
                P.ts(orst[:, hh, :nt], psO[:, hh * 128:hh * 128 + nt], 1.0 / 128, 1e-6, ALU.mult, ALU.add, ['psO'], ['orst'])
            P.act(orst[:, :, :nt], orst[:, :, :nt], AF.Sqrt, ['orst'], ['orst'])
            P.op('dve', lambda e: e.reciprocal(orst[:, :, :nt], orst[:, :, :nt]), ['orst'], ['orst'])
            P.act(gs[:, :, :nt], gs[:, :, :nt], AF.Silu, ['gs'], ['gs'])
            P.tt(o1[:, :, :nt], o1[:, :, :nt], orst[:, :, :nt], ALU.mult, ['o1', 'orst'], ['o1'])
            for hh in range(8):
                P.stt(ms[:, 8 + hh, :nt], o1[:, hh, :nt], hnw[:, 0:1], gs[:, hh, :nt], ALU.mult, ALU.mult,
                      ['o1', 'hnw', 'gs'], ['ms'])
        else:
            P.ld(ms[:, :, :nt], mTd.rearrange("(k p) t -> p k t", p=128)[:, :, t0:t0 + nt], [], ['ms'])
        P.cp(mb[:, :, :nt], ms[:, :, :nt], ['ms'], ['mb'], eng='pool')
        P.ld(xs[:nt, :], x[t0:t0 + nt, :], [], ['xs'])
        for n in range(4):
            for k in range(16):
                P.mm(psA[:nt, n * 512:(n + 1) * 512], mb[:, k, :nt], wb[:, k, n * 512:(n + 1) * 512], k == 0, k == 15,
                     ['mb', 'wb'], [A[n]])
        P.tt(x1[:nt, :], psA[:nt, :], bc[:nt, 0, :], ALU.mult, A + ['bc'], ['x1'])
        P.tt(x1[:nt, :], x1[:nt, :], xs[:nt, :], ALU.add, ['x1', 'xs'], ['x1'])
        P.ld(x1o[t0:t0 + nt, :], x1[:nt, :], ['x1'], ['x1o'], eng='act')
        P.act(junk[:nt, :], x1[:nt, :], AF.Square, ['x1'], ['junk', 'ssq'], accum_out=ssq[:nt, :])
        P.ts(ssq[:nt, :], ssq[:nt, :], 1.0 / 2048, 1e-6, ALU.mult, ALU.add, ['ssq'], ['ssq'])
        P.act(ssq[:nt, :], ssq[:nt, :], AF.Sqrt, ['ssq'], ['ssq'])
        P.op('dve', lambda e: e.reciprocal(rstd[:nt, :], ssq[:nt, :]), ['ssq'], ['rstd'])
        P.stt(h2[:nt, :], x1[:nt, :], rstd[:nt, 0:1], bc[:nt, 2, :], ALU.mult, ALU.mult, ['x1', 'rstd', 'bc'], ['h2'])
        P.tt(h2[:nt, :], h2[:nt, :], bc[:nt, 3, :], ALU.add, ['h2', 'bc'], ['h2'], eng='pool')
        P.ld(h2o[t0:t0 + nt, :], h2[:nt, :], ['h2'], ['h2o'], eng='act')

    t0 = 0
    for ti, nt in enumerate(tiles):
        tile_body(ti, nt, t0)
        t0 += nt
    return P


def rms(x, w):
    return x / np.sqrt((x * x).mean(-1, keepdims=True) + 1e-6) * w


def conv_tiles():
    t = [(0, 64, 0)]
    for i in range(4):
        t.append((94 + 512 * i, 512, 64 + 512 * i))
    return t


def build_conv(tiles, Lext, T):
    P = Prog()
    zx = P.inp("zx", [3072, Lext])
    dwd = P.inp("dw", [128, 8, 31])
    lnwd = P.inp("lnw", [128, 8])
    lnbd = P.inp("lnb", [128, 8])
    pwd = P.inp("pw", [128, 4, 2, 256])
    pscd = P.inp("pscale", [128, 8])
    icd = P.inp("invcnt", [4, T])
    mT = P.outp("mT", [2048, T])
    ones = P.sb("ones", [128, 128])
    dw = P.sb("dw_sb", [128, 8, 31])
    lnw = P.sb("lnw_sb", [128, 8])
    lnb = P.sb("lnb_sb", [128, 8])
    pws = P.sb("pws", [128, 4, 2, 256])
    pwb = P.sb("pwb", [128, 4, 2, 256], BF16)
    psc = P.sb("psc", [128, 8])
    ic = P.sb("ic", [128, 4, 512])
    at = [P.sb("at%d" % i, [128, 544]) for i in range(2)]
    bt = [P.sb("bt%d" % i, [128, 544]) for i in range(2)]
    ut = [P.sb("ut%d" % i, [128, 544]) for i in range(2)]
    cv = P.sb("cv", [128, 8, 512])
    sq = [P.sb("sq%d" % i, [128, 512]) for i in range(2)]
    mu = P.sb("mu", [128, 512])
    var = P.sb("var", [128, 512])
    tmp = [P.sb("tmp%d" % i, [128, 512]) for i in range(2)]
    mo = P.sb("mo", [128, 16, 512])
    pt = [P.sb("pt%d" % i, [128, 544]) for i in range(2)]
    sa = [P.sb("sa%d" % i, [128, 544]) for i in range(2)]
    sb_ = [P.sb("sb%d" % i, [128, 544]) for i in range(2)]
    plT = P.sb("plT", [128, 8, 512], BF16)
    ps1 = P.ps("ps1")
    ps2 = P.ps("ps2")
    psg = [P.ps("psg%d" % i) for i in range(2)]
    P.memset(ones[:], 1.0, ['ones'])
    P.ld(dw[:], dwd, [], ['dw'])
    P.ld(lnw[:], lnwd, [], ['lnw'])
    P.ld(lnb[:], lnbd, [], ['lnb'])
    P.ld(pws[:], pwd, [], ['pws'])
    P.cp(pwb[:], pws[:], ['pws'], ['pwb'])
    P.ld(psc[:], pscd, [], ['psc'])

    def tile_body(e0, n, o0):
        ne = n + 30
        for g in range(4):
            P.ld(ic[:, g, :n], icd[g:g + 1, o0:o0 + n].partition_broadcast(128), [], ['ic'])
        for k in range(8):
            b2 = k % 2
            P.ld(at[b2][:, :ne], zx[k * 128:(k + 1) * 128, e0:e0 + ne], [], ['at%d' % b2])
            P.ld(bt[b2][:, :ne], zx[1024 + k * 128:1024 + (k + 1) * 128, e0:e0 + ne], [], ['bt%d' % b2])
            P.act(bt[b2][:, :ne], bt[b2][:, :ne], AF.Sigmoid, ['bt%d' % b2], ['bt%d' % b2])
            P.tt(ut[b2][:, :ne], at[b2][:, :ne], bt[b2][:, :ne], ALU.mult, ['at%d' % b2, 'bt%d' % b2], ['ut%d' % b2], eng='pool')
            ck = ('cv', k)
            P.ts(cv[:, k, :n], ut[b2][:, 0:n], dw[:, k, 0:1], None, ALU.mult, None, ['ut%d' % b2, 'dw'], [ck])
            for j in range(1, 31):
                P.stt(cv[:, k, :n], ut[b2][:, j:j + n], dw[:, k, j:j + 1], cv[:, k, :n], ALU.mult, ALU.add,
                      ['ut%d' % b2, 'dw', ck], [ck])
            P.act(sq[b2][:, :n], cv[:, k, :n], AF.Square, [ck], ['sq%d' % b2])
            P.mm(ps1[:, :n], ones[:], cv[:, k, :n], k == 0, k == 7, ['ones', ck], ['ps1'])
            P.mm(ps2[:, :n], ones[:], sq[b2][:, :n], k == 0, k == 7, ['ones', 'sq%d' % b2], ['ps2'])
        P.ts(mu[:, :n], ps1[:, :n], 1.0 / 1024, None, ALU.mult, None, ['ps1'], ['mu'])
        P.tt(var[:, :n], mu[:, :n], mu[:, :n], ALU.mult, ['mu'], ['var'])
        P.stt(var[:, :n], ps2[:, :n], 1.0 / 1024, var[:, :n], ALU.mult, ALU.subtract, ['ps2', 'var'], ['var'])
        P.ts(var[:, :n], var[:, :n], 1e-6, None, ALU.add, None, ['var'], ['var'])
        P.act(var[:, :n], var[:, :n], AF.Sqrt, ['var'], ['var'])
        P.op('dve', lambda e: e.reciprocal(var[:, :n], var[:, :n]), ['var'], ['var'])
        for k in range(8):
            b2 = k % 2
            eng = 'dve' if b2 == 0 else 'pool'
            P.tt(tmp[b2][:, :n], cv[:, k, :n], mu[:, :n], ALU.subtract, [('cv', k), 'mu'], ['tmp%d' % b2], eng=eng)
            P.tt(tmp[b2][:, :n], tmp[b2][:, :n], var[:, :n], ALU.mult, ['tmp%d' % b2, 'var'], ['tmp%d' % b2], eng=eng)
            P.act(mo[:, k, :n], tmp[b2][:, :n], AF.Silu, ['tmp%d' % b2, 'lnw', 'lnb'], ['moA'],
                  bias=lnb[:, k:k + 1], scale=lnw[:, k:k + 1])
        P.ld(mT.rearrange("(k p) t -> p k t", p=128)[:, 0:8, o0:o0 + n], mo[:, 0:8, :n], ['moA'], ['mT'], eng='act')
        for gi in range(4):
            wlog = gi + 1
            wn = 2 ** wlog
            for cc in range(2):
                kk = 2 * gi + cc
                b2 = kk % 2
                r0 = 2048 + kk * 128
                P.ld(pt[b2][:, :ne], zx[r0:r0 + 128, e0:e0 + ne], [], ['pt%d' % b2])
                cur, curname, L = pt[b2], 'pt%d' % b2, ne
                step = 1
                for lv in range(wlog):
                    dst = (sa, sb_)[lv % 2][b2]
                    dname = ('sa%d', 'sb%d')[lv % 2] % b2
                    L2 = L - step
                    P.tt(dst[:, :L2], cur[:, 0:L2], cur[:, step:step + L2], ALU.add, [curname], [dname],
                         eng='pool' if b2 else 'dve')
                    cur, curname, L = dst, dname, L2
                    step *= 2
                s0 = 15 - wn // 2
                tm = tmp[b2]
                P.tt(tm[:, :n], cur[:, s0:s0 + n], ic[:, gi, :n], ALU.mult, [curname, 'ic'], ['tmp%d' % b2])
                P.tt(plT[:, kk, :n], tm[:, :n], pt[b2][:, 15:15 + n], ALU.subtract, ['tmp%d' % b2, 'pt%d' % b2], [('pl', kk)])
        for gi in range(4):
            for db in range(2):
                pb = (gi * 2 + db) % 2
                for cc in range(2):
                    P.mm(psg[pb][:, :n], pwb[:, gi, cc, db * 128:(db + 1) * 128], plT[:, 2 * gi + cc, :n], cc == 0, cc == 1,
                         ['pwb', ('pl', 2 * gi + cc)], ['psg%d' % pb])
                P.ts(mo[:, 8 + gi * 2 + db, :n], psg[pb][:, :n], psc[:, gi * 2 + db:gi * 2 + db + 1], None, ALU.mult, None,
                     ['psg%d' % pb, 'psc'], ['moB'])
        P.ld(mT.rearrange("(k p) t -> p k t", p=128)[:, 8:16, o0:o0 + n], mo[:, 8:16, :n], ['moB'], ['mT'], eng='act')

    for (e0, n, o0) in tiles:
        tile_body(e0, n, o0)
    return P


def col8(a):
    return np.ascontiguousarray(a.reshape(-1, 128).T)


def conv_host_consts(conv_dw, ln_w, ln_b, pool_w, pool_scale):
    dw = np.ascontiguousarray(conv_dw.T.reshape(8, 128, 31).transpose(1, 0, 2))
    pw = np.ascontiguousarray(pool_w.reshape(4, 2, 128, 256).transpose(2, 0, 1, 3))
    return {"dw": dw, "lnw": col8(ln_w), "lnb": col8(ln_b), "pw": pw, "pscale": col8(pool_scale)}


def invcnt_for(seg_len, t_lo, n):
    out = np.zeros((4, n), np.float32)
    t = np.arange(t_lo, t_lo + n)
    for gi, w in enumerate((2, 4, 8, 16)):
        lo = np.clip(t - w // 2, 0, seg_len)
        hi = np.clip(t + w - w // 2, 0, seg_len)
        out[gi] = 1.0 / (hi - lo).astype(np.float32)
    return out


LTOT = 8448
NCTX = 256
LLAT = 8192
CH = 32


def mix_consts():
    f = np.float64
    c = {}
    i256 = np.arange(256, dtype=f)
    a256 = 2 * np.pi * np.outer(i256, i256) / 256
    C256, S256 = np.cos(a256), np.sin(a256)
    lay = lambda m: np.ascontiguousarray(m.reshape(2, 128, 256).transpose(1, 0, 2)).astype(np.float32)
    c["c256"] = lay(C256)
    c["s256n"] = lay(-S256)
    c["cp256"] = lay(C256 / 256.0)
    c["sp256"] = lay(S256 / 256.0)
    i128 = np.arange(128, dtype=f)
    a128 = 2 * np.pi * np.outer(i128, i128) / 128
    c["c128"] = np.cos(a128).astype(np.float32)
    c["s128"] = np.sin(a128).astype(np.float32)
    c["s128n"] = (-np.sin(a128)).astype(np.float32)
    i64 = np.arange(64, dtype=f)
    atw = 2 * np.pi * np.outer(i64, i128) / 8192
    c["twc"] = np.cos(atw).astype(np.float32)
    c["tws"] = np.sin(atw).astype(np.float32)
    a64 = 2 * np.pi * np.outer(i64, i64) / 64
    sc = 1.0 / np.sqrt(8192.0 * 256.0)
    c["c64s"] = (np.cos(a64) * sc).astype(np.float32)
    c["s64s"] = (np.sin(a64) * sc).astype(np.float32)
    c["maskT"] = np.triu(np.ones((64, 64), np.float32))
    c["maskI"] = np.triu(np.ones((CH, CH), np.int32)).astype(np.int32)
    c["ident"] = np.eye(128, dtype=np.float32)
    rm = np.ones((128, 256), np.float32)
    rm[:, ::CH] = 0
    c["rmask"] = rm
    return c


def build_mix(layer_j, do_four=True, do_hg=True, nsb=33):
    P = Prog()
    NCH = 256 // CH
    aT = P.inp("aT", [256, LTOT])
    qTd = P.inp("qT", [4, 128, LTOT])
    flTd = P.inp("flT", [4, 128, LTOT])
    vvd = P.inp("vv", [4, LTOT, 128])
    lbld = P.inp("lbl", [128, 2, 2])
    cd = {}
    shapes = {"c256": [128, 2, 256], "s256n": [128, 2, 256], "cp256": [128, 2, 256], "sp256": [128, 2, 256],
              "c128": [128, 128], "s128": [128, 128], "s128n": [128, 128], "twc": [64, 128], "tws": [64, 128],
              "c64s": [64, 64], "s64s": [64, 64], "maskT": [64, 64], "ident": [128, 128], "rmask": [128, 256]}
    for k, s in shapes.items():
        cd[k] = P.inp(k, s)
    maskId = P.inp("maskI", [CH, CH], I32)
    fourT = P.outp("fourT", [256, LTOT])
    oo = P.outp("oo", [4, LTOT, 128])
    pb = [P.ps("pb%d" % i) for i in range(7)]
    pbh = P.ps("pb7", [128, 1024], BF16)
    cs = {}
    cb = {}
    for k, s in shapes.items():
        cs[k] = P.sb("cs_" + k, s)
        P.ld(cs[k][:], cd[k], [], ['cs_' + k])
        if k in ("c256", "s256n", "cp256", "sp256", "c128", "s128", "s128n", "c64s", "s64s", "ident"):
            cb[k] = P.sb("cb_" + k, s, BF16)
            P.cp(cb[k][:], cs[k][:], ['cs_' + k], ['cb_' + k])

    if do_four:
        ast = P.sb("ast", [128, 2112])
        ab = P.sb("ab", [128, 2, LTOT], BF16)
        av = aT.rearrange("(c p) t -> p c t", p=128)
        for cc in range(2):
            for q in range(4):
                P.ld(ast[:], av[:, cc, q * 2112:(q + 1) * 2112], [], ['ast'])
                P.cp(ab[:, cc, q * 2112:(q + 1) * 2112], ast[:], ['ast'], ['ab'], eng='pool' if q % 2 else 'act')
        yc = P.sb("yc", [128, 2, 2, 256], BF16)
        for blk in range(2):
            for ri, cn in enumerate(("c256", "s256n")):
                for cc in range(2):
                    P.mm(pb[ri][:, 0:256], ab[:, cc, blk * 128:(blk + 1) * 128], cb[cn][:, cc, :], cc == 0, cc == 1,
                         ['ab', 'cb_' + cn], ['pb%d' % ri])
                P.cp(yc[:, blk, ri, :], pb[ri][:, 0:256], ['pb%d' % ri], ['yc'], eng='act' if ri else 'dve')
        octx = P.sb("octx", [128, 2, 256])
        for kb in range(2):
            i = 0
            for blk in range(2):
                for ri, cn in enumerate(("cp256", "sp256")):
                    P.mm(pb[2][:, 0:256], yc[:, blk, ri, kb * 128:(kb + 1) * 128], cb[cn][:, blk, :], i == 0, i == 3,
                         ['yc', 'cb_' + cn], ['pb2'])
                    i += 1
            P.cp(octx[:, kb, :], pb[2][:, 0:256], ['pb2'], ['octx'])
        P.ld(fourT.rearrange("(k p) t -> p k t", p=128)[:, :, 0:256], octx[:], ['octx'], ['fourT'], eng='act')
        Dr = P.sb("Dr", [128, 64, 128], BF16)
        Di = P.sb("Di", [128, 64, 128], BF16)
        Br = [P.sb("Br%d" % i, [64, 4, 128], BF16) for i in range(2)]
        Bi = [P.sb("Bi%d" % i, [64, 4, 128], BF16) for i in range(2)]
        t1 = P.sb("t1", [64, 4, 128])
        t2 = P.sb("t2", [64, 4, 128])
        ostg = [P.sb("fostg%d" % i, [64, 4, 128]) for i in range(2)]
        twc4 = cs["twc"][:].unsqueeze(1).to_broadcast([64, 4, 128])
        tws4 = cs["tws"][:].unsqueeze(1).to_broadcast([64, 4, 128])
        for kh in range(2):
            for g in range(16):
                for ri, cn, Dd, dn in ((0, "c256", Dr, 'Dr'), (1, "s256n", Di, 'Di')):
                    pbi = 2 * (g % 2) + ri
                    for q in range(4):
                        nlo = g * 4 + q
                        for cc in range(2):
                            P.mm(pb[pbi][:, q * 128:(q + 1) * 128], ab[:, cc, NCTX + nlo:NCTX + LLAT:64],
                                 cb[cn][:, cc, kh * 128:(kh + 1) * 128], cc == 0, cc == 1, ['ab', 'cb_' + cn], ['pb%d' % pbi])
                    P.cp(Dd[:, g * 4:(g + 1) * 4, :].rearrange("p a b -> p (a b)"), pb[pbi][:, :], ['pb%d' % pbi], [dn],
                         eng='act' if ri else 'dve')
            for cg in range(32):
                b2 = cg % 2
                par, pai, px = pb[4], pb[5], pb[6]
                for q in range(4):
                    ch = cg * 4 + q
                    P.mm(par[0:64, q * 128:(q + 1) * 128], Dr[:, :, ch], cb["c128"][:], True, False, ['Dr', 'cb_c128'], ['pb4'])
                    P.mm(par[0:64, q * 128:(q + 1) * 128], Di[:, :, ch], cb["s128"][:], False, True, ['Di', 'cb_s128'], ['pb4'])
                    P.mm(pai[0:64, q * 128:(q + 1) * 128], Di[:, :, ch], cb["c128"][:], True, False, ['Di', 'cb_c128'], ['pb5'])
                    P.mm(pai[0:64, q * 128:(q + 1) * 128], Dr[:, :, ch], cb["s128n"][:], False, True, ['Dr', 'cb_s128n'], ['pb5'])
                ar3 = par[0:64, :].rearrange("p (a b) -> p a b", b=128)
                ai3 = pai[0:64, :].rearrange("p (a b) -> p a b", b=128)
                P.tt(t1[:], ar3, twc4, ALU.mult, ['pb4', 'cs_twc'], ['t1'])
                P.tt(t2[:], ai3, tws4, ALU.mult, ['pb5', 'cs_tws'], ['t2'])
                P.tt(Br[b2][:], t1[:], t2[:], ALU.add, ['t1', 't2'], ['Br%d' % b2], eng='pool')
                P.tt(t1[:], ai3, twc4, ALU.mult, ['pb5', 'cs_twc'], ['t1'])
                P.tt(t2[:], ar3, tws4, ALU.mult, ['pb4', 'cs_tws'], ['t2'])
                P.tt(Bi[b2][:], t1[:], t2[:], ALU.subtract, ['t1', 't2'], ['Bi%d' % b2], eng='pool')
                for q in range(4):
                    P.mm(px[0:64, q * 128:(q + 1) * 128], cb["c64s"][:], Br[b2][:, q, :], True, False, ['cb_c64s', 'Br%d' % b2], ['pb6'])
                    P.mm(px[0:64, q * 128:(q + 1) * 128], cb["s64s"][:], Bi[b2][:, q, :], False, True, ['cb_s64s', 'Bi%d' % b2], ['pb6'])
                P.cp(ostg[b2][:].rearrange("p a b -> p (a b)"), px[0:64, :], ['pb6'], ['fostg%d' % b2], eng='act')
                c0 = kh * 128 + cg * 4
                P.ld(fourT[c0:c0 + 4, NCTX:].rearrange("c (k2 k1) -> k2 c k1", k1=128), ostg[b2][:], ['fostg%d' % b2], ['fourT'],
                     eng='act')

    if do_hg:
        maskI = P.sb("maskI_sb", [CH, CH], I32)
        P.ld(maskI[:], maskId, [], ['maskI'])
        lbl = P.sb("lbl_sb", [128, 2, 2])
        lb = P.sb("lb", [128, 2])
        oml = P.sb("oml", [128, 2])
        P.ld(lbl[:], lbld, [], ['lbl'])
        if layer_j == 0:
            P.memset(lb[:], 0.0, ['lb'])
        else:
            P.tt(lb[:], lbl[:, :, 1], lbl[:, :, 0], ALU.subtract, ['lbl'], ['lb'])
            P.act(lb[:], lb[:], AF.Sigmoid, ['lb'], ['lb'])
        P.ts(oml[:], lb[:], -1.0, 1.0, ALU.mult, ALU.add, ['lb'], ['oml'])
        NS = 4
        S32 = [P.sb("S32_%d" % s, [128, 128]) for s in range(NS)]
        Sb = [P.sb("Sb_%d" % s, [128, 128], BF16) for s in range(NS)]
        for s in range(NS):
            P.memset(S32[s][:], 0.0, ['S32_%d' % s])
            P.memset(Sb[s][:], 0.0, ['Sb_%d' % s], eng='pool')

        def mk(nm, shape, dt=F32, n=2):
            return [[P.sb("%s_%d_%d" % (nm, s, i), shape, dt) for i in range(n)] for s in range(NS)]
        qs_ = mk("qs", [128, 256])
        fl_ = mk("fl", [128, 256])
        lf_ = mk("lf", [128, 256])
        kk_ = mk("kk", [128, 256])
        bb_ = mk("bb", [128, 256])
        vs_ = mk("vs", [CH, NCH, 128], n=1)
        vb_ = mk("vb", [CH, NCH, 128], BF16)
        os_ = mk("os", [CH, NCH, 128], n=1)
        nbm_ = mk("nbm", [128, NCH])
        ex = [[P.sb("ex_%d_%d" % (s, i), [128, CH]) for i in range(4)] for s in range(NS)]
        Qt = mk("Qt", [128, CH], BF16)
        Kt = mk("Kt", [128, CH], BF16)
        Qs = mk("Qs", [128, CH], BF16)
        Kp = mk("Kp", [128, CH], BF16)
        ATs = mk("ATs", [CH, CH], BF16)
        Kps = mk("Kps", [CH, 128], BF16)
        for s in range(NS):
            for i in range(2):
                P.memset(ATs[s][i][:], 0.0, ["ATs_%d_%d" % (s, i)], eng='pool')

        def nm(base, s, i):
            return "%s_%d_%d" % (base, s, i)

        for sbi in range(nsb):
            p2 = sbi % 2
            c0 = sbi * 256
            for s in range(NS):
                hh = s // 2
                P.ld(qs_[s][p2][:], qTd[s, :, c0:c0 + 256], [], [nm('qs', s, p2)])
                P.ld(fl_[s][p2][:], flTd[s, :, c0:c0 + 256], [], [nm('fl', s, p2)])
                P.ld(vs_[s][0][:], vvd[s, c0:c0 + 256, :].rearrange("(c p) d -> p c d", p=CH), [], [nm('vs', s, 0)])
                P.cp(vb_[s][p2][:], vs_[s][0][:], [nm('vs', s, 0)], [nm('vb', s, p2)], eng='pool')
                P.act(qs_[s][p2][:], qs_[s][p2][:], AF.Silu, [nm('qs', s, p2)], [nm('qs', s, p2)])
                P.act(fl_[s][p2][:], fl_[s][p2][:], AF.Sigmoid, [nm('fl', s, p2)], [nm('fl', s, p2)])
                P.ts(fl_[s][p2][:], fl_[s][p2][:], oml[:, hh:hh + 1], lb[:, hh:hh + 1], ALU.mult, ALU.add,
                     [nm('fl', s, p2), 'oml', 'lb'], [nm('fl', s, p2)])
                P.ts(kk_[s][p2][:], fl_[s][p2][:], -1.0, 1.0, ALU.mult, ALU.add, [nm('fl', s, p2)], [nm('kk', s, p2)], eng='pool')
                P.ts(fl_[s][p2][:], fl_[s][p2][:], 1e-30, None, ALU.max, None, [nm('fl', s, p2)], [nm('fl', s, p2)])
                P.act(lf_[s][p2][:], fl_[s][p2][:], AF.Ln, [nm('fl', s, p2)], [nm('lf', s, p2)])
                P.op('dve', lambda e, s=s, p2=p2: e.tensor_tensor_scan(bb_[s][p2][:], cs["rmask"][:], lf_[s][p2][:], 0.0,
                                                                       ALU.mult, ALU.add),
                     ['cs_rmask', nm('lf', s, p2)], [nm('bb', s, p2)])
                P.ts(nbm_[s][p2][:], bb_[s][p2][:, CH // 2 - 1:256:CH], -1.0, None, ALU.mult, None, [nm('bb', s, p2)], [nm('nbm', s, p2)])
            for ci in range(NCH):
                gc = sbi * NCH + ci
                c2 = gc % 2
                sl = slice(ci * CH, (ci + 1) * CH)
                for s in range(NS):
                    b_c = bb_[s][p2][:, sl]
                    bmid = bb_[s][p2][:, ci * CH + CH // 2 - 1:ci * CH + CH // 2]
                    blast = bb_[s][p2][:, ci * CH + CH - 1:ci * CH + CH]
                    nb = nbm_[s][p2][:, ci:ci + 1]
                    rb = [nm('bb', s, p2), nm('nbm', s, p2)]
                    exn = ['ex_%d_%d' % (s, i) for i in range(4)]
                    P.act(ex[s][0][:], b_c, AF.Exp, rb, [exn[0]], bias=nb, scale=1.0)
                    P.act(ex[s][1][:], b_c, AF.Exp, rb, [exn[1]], bias=bmid, scale=-1.0)
                    P.act(ex[s][2][:], b_c, AF.Exp, rb, [exn[2]])
                    P.act(ex[s][3][:], b_c, AF.Exp, rb, [exn[3]], bias=blast, scale=-1.0)
                    qn, kn = nm('qs', s, p2), nm('kk', s, p2)
                    P.tt(Qt[s][c2][:], qs_[s][p2][:, sl], ex[s][0][:], ALU.mult, [qn, exn[0]], [nm('Qt', s, c2)])
                    P.tt(Kt[s][c2][:], kk_[s][p2][:, sl], ex[s][1][:], ALU.mult, [kn, exn[1]], [nm('Kt', s, c2)], eng='pool')
                    P.tt(Qs[s][c2][:], qs_[s][p2][:, sl], ex[s][2][:], ALU.mult, [qn, exn[2]], [nm('Qs', s, c2)])
                    P.tt(Kp[s][c2][:], kk_[s][p2][:, sl], ex[s][3][:], ALU.mult, [kn, exn[3]], [nm('Kp', s, c2)], eng='pool')
                    pa = 'pb%d' % (s % 2)
                    P.mm(pb[s % 2][0:CH, 0:CH], Kt[s][c2][:], Qt[s][c2][:], True, True, [nm('Kt', s, c2), nm('Qt', s, c2)], [pa])
                    P.op('dve', lambda e, s=s, c2=c2: e.copy_predicated(ATs[s][c2][:], maskI[:], pb[s % 2][0:CH, 0:CH]),
                         [pa, 'maskI', nm('ATs', s, c2)], [nm('ATs', s, c2)])
                    P.tr(pbh[0:CH, (s % 2) * 128:(s % 2) * 128 + 128], Kp[s][c2][:], cb["ident"][:], [nm('Kp', s, c2), 'cb_ident'],
                         ['pb7_%d' % (s % 2)])
                    P.cp(Kps[s][c2][:], pbh[0:CH, (s % 2) * 128:(s % 2) * 128 + 128], ['pb7_%d' % (s % 2)], [nm('Kps', s, c2)], eng='act')
                    po = 'pb%d' % (2 + s % 2)
                    P.mm(pb[2 + s % 2][0:CH, 0:128], ATs[s][c2][:], vb_[s][p2][:, ci, :], True, False,
                         [nm('ATs', s, c2), nm('vb', s, p2)], [po])
                    P.mm(pb[2 + s % 2][0:CH, 0:128], Qs[s][c2][:], Sb[s][:], False, True, [nm('Qs', s, c2), 'Sb_%d' % s], [po])
                    P.cp(os_[s][0][:, ci, :], pb[2 + s % 2][0:CH, 0:128], [po], [nm('os', s, 0)], eng='act')
                    pS = 'pb%d' % (4 + s % 2)
                    P.mm(pb[4 + s % 2][:, 0:128], Kps[s][c2][:], vb_[s][p2][:, ci, :], True, True,
                         [nm('Kps', s, c2), nm('vb', s, p2)], [pS])
                    P.stt(S32[s][:], S32[s][:], ex[s][2][:, CH - 1:CH], pb[4 + s % 2][:, 0:128], ALU.mult, ALU.add,
                          ['S32_%d' % s, exn[2], pS], ['S32_%d' % s])
                    P.cp(Sb[s][:], S32[s][:], ['S32_%d' % s], ['Sb_%d' % s], eng='pool')
            for s in range(NS):
                P.ld(oo[s, c0:c0 + 256, :].rearrange("(c p) d -> p c d", p=CH), os_[s][0][:], [nm('os', s, 0)], ['oo'], eng='act')
    return P


def hgrn_ref(q, fl, v, lb):
    L = q.shape[0]
    sig = 1 / (1 + np.exp(-fl))
    f = lb + (1 - lb) * sig
    k = 1 - f
    qs = q / (1 + np.exp(-q))
    S = np.zeros((128, 128))
    o = np.zeros((L, 128))
    for t in range(L):
        S = f[t][:, None] * S + np.outer(k[t], v[t])
        o[t] = S.T @ qs[t]
    return o


_PROGS = {}


def _prog(key, fn):
    if key not in _PROGS:
        _PROGS[key] = fn().build()
    return _PROGS[key]


def build_add():
    P = Prog()
    xa = P.inp("xa", [2048, 2048])
    pe = P.inp("pe", [2048, 2048])
    out = P.outp("xo", [2048, 2048])
    a = [P.sb("a%d" % i, [128, 2048]) for i in range(2)]
    b = [P.sb("b%d" % i, [128, 2048]) for i in range(2)]
    for i in range(16):
        k = i % 2
        P.ld(a[k][:], xa[i * 128:(i + 1) * 128, :], [], ['a%d' % k])
        P.ld(b[k][:], pe[i * 128:(i + 1) * 128, :], [], ['b%d' % k])
        P.tt(a[k][:], a[k][:], b[k][:], ALU.add, ['a%d' % k, 'b%d' % k], ['a%d' % k], eng='dve' if k == 0 else 'pool')
        P.ld(out[i * 128:(i + 1) * 128, :], a[k][:], ['a%d' % k], ['out'], eng='act')
    return P


def _sincos_table():
    quarter = 512
    row = np.repeat(np.arange(128), 64).astype(np.float32)[:, None]
    colv = np.tile(np.arange(64), 128).astype(np.float32)[:, None]
    omega = (1.0 / (np.float32(10000.0) ** (np.arange(quarter, dtype=np.float32) / np.float32(quarter)))).astype(np.float32)
    ar, ac = (row * omega).astype(np.float32), (colv * omega).astype(np.float32)
    return np.concatenate([np.sin(ar), np.cos(ar), np.sin(ac), np.cos(ac)], axis=-1).astype(np.float32)


def _col16(a):
    return np.ascontiguousarray(np.asarray(a, np.float32).reshape(16, 128).T)


def _flipseg(a):
    return np.concatenate([a[..., :256][..., ::-1], a[..., 256:][..., ::-1]], axis=-1)


def _run(nc, maps):
    res = run_bass_kernel_spmd(nc, maps, core_ids=list(range(8)))
    return res.results


def kernel(x, c, ctx, c_ctx, ada_w, ada_b, norm1_w, norm2_w, final_norm_w, ev_in_w, ev_out_w, hg_lb_logits, hg_norm_w,
           od_in_w, od_out_w, conv_dw, conv_ln_w, conv_ln_b, pool_w, pool_scale, peer_q_w, peer_keys, peer_u, peer_v):
    f32 = lambda a: np.ascontiguousarray(np.asarray(a, dtype=np.float32))
    x, c, ctx, c_ctx = f32(x), f32(c), f32(ctx), f32(c_ctx)
    ada_w, ada_b = np.asarray(ada_w, np.float32), np.asarray(ada_b, np.float32)
    cores = [(b, s) for b in range(2) for s in range(4)]
    TT = [64, 512, 512, 512, 512]
    T128 = [64] + [128] * 16
    pe = _sincos_table()
    nc = _prog('add', build_add)
    r = _run(nc, [{"xa": f32(x[b, 2048 * s:2048 * (s + 1)]), "pe": f32(pe[2048 * s:2048 * (s + 1)])} for (b, s) in cores])
    X = [np.concatenate([ctx[b, 64 * s:64 * (s + 1)], r[i]["xo"]], axis=0) for i, (b, s) in enumerate(cores)]
    cv = np.stack([c[0], c[1], c_ctx], 0)
    cT = np.ascontiguousarray(cv.T.reshape(16, 128, 3).transpose(1, 0, 2))
    nc = _prog('mod', build_mod)
    maps = []
    for i in range(8):
        l, hf = i // 2, i % 2
        maps.append({"cT": cT, "w": f32(ada_w[l][:, hf * 6144:(hf + 1) * 6144]),
                     "b": f32(ada_b[l][None, hf * 6144:(hf + 1) * 6144])})
    r = _run(nc, maps)
    mod = [np.concatenate([r[2 * l]["mod"], r[2 * l + 1]["mod"]], axis=1).reshape(3, 6, 2048) for l in range(4)]
    mc = mix_consts()
    pc = peer_consts()
    for l in range(4):
        j = l // 2
        even = (l % 2 == 0)
        N = 6144 if even else 3072
        w_in = f32(ev_in_w[j] if even else od_in_w[j])
        w_out = f32(ev_out_w[j] if even else od_out_w[j])
        nc = _prog(('pre', N), lambda: build_pre(TT, N))
        maps = []
        for (b, s), Xc in zip(cores, X):
            mods = np.zeros((128, 2, 2, 16), np.float32)
            for rr, mv in ((0, mod[l][2]), (1, mod[l][b])):
                mods[:, rr, 0, :] = _col16(mv[0])
                mods[:, rr, 1, :] = _col16(mv[1])
            maps.append({"xT": np.ascontiguousarray(Xc.T), "w": w_in, "nw": _col16(norm1_w[l]), "mods": mods})
        r = _run(nc, maps)
        ZT = []
        for b in range(2):
            zc = [r[4 * b + s]["zT"] for s in range(4)]
            ZT.append(np.concatenate([z[:, :64] for z in zc] + [z[:, 64:] for z in zc], axis=1))
        if even:
            nc = _prog(('mix', j), lambda: build_mix(j))
            maps = []
            for (b, s) in cores:
                Z = ZT[b]
                qs, fls, vs = [], [], []
                for hh in range(2):
                    h = 2 * s + hh
                    q_ = Z[1024 + 128 * h:1024 + 128 * (h + 1)]
                    ff = Z[2048 + 128 * h:2048 + 128 * (h + 1)]
                    fb = Z[3072 + 128 * h:3072 + 128 * (h + 1)]
                    v_ = Z[4096 + 128 * h:4096 + 128 * (h + 1)]
                    qs += [q_, _flipseg(q_)]
                    fls += [ff, _flipseg(fb)]
                    vs += [v_.T, _flipseg(v_).T]
                lbl = np.zeros((128, 2, 2), np.float32)
                for hh in range(2):
                    h = 2 * s + hh
                    for rr in range(2):
                        lbl[:, hh, rr] = hg_lb_logits[rr, 128 * h:128 * (h + 1)]
                m = {"aT": f32(Z[256 * s:256 * (s + 1)]), "qT": f32(np.stack(qs, 0)), "flT": f32(np.stack(fls, 0)),
                     "vv": f32(np.stack(vs, 0)), "lbl": lbl}
                m.update(mc)
                maps.append(m)
            r = _run(nc, maps)
            mixin = []
            for (b, s) in cores:
                cols = np.r_[64 * s:64 * (s + 1), 256 + 2048 * s:256 + 2048 * (s + 1)]
                F = np.concatenate([r[4 * b + s2]["fourT"] for s2 in range(4)], axis=0)[:, cols]
                of, ob = [], []
                for h in range(8):
                    oo = r[4 * b + h // 2]["oo"]
                    hh = h % 2
                    of.append(oo[hh * 2].T[:, cols])
                    ob.append(_flipseg(oo[hh * 2 + 1].T)[:, cols])
                mixin.append({"fourT": f32(F), "ofT": f32(np.concatenate(of, 0)), "obT": f32(np.concatenate(ob, 0)),
                              "gT": f32(ZT[b][5120:6144][:, cols]), "hnw": f32(np.asarray(hg_norm_w[j]).reshape(128, 1))})
        else:
            nc = _prog('conv', lambda: build_conv(conv_tiles(), 2172, 2112))
            hc = conv_host_consts(np.asarray(conv_dw[j], np.float32), np.asarray(conv_ln_w[j], np.float32),
                                  np.asarray(conv_ln_b[j], np.float32), np.asarray(pool_w[j], np.float32),
                                  np.asarray(pool_scale[j], np.float32))
            maps = []
            for (b, s) in cores:
                Z = ZT[b]
                zc = np.pad(Z[:, :256], ((0, 0), (15, 15)))[:, 64 * s:64 * s + 94]
                zl = np.pad(Z[:, 256:], ((0, 0), (15, 15)))[:, 2048 * s:2048 * s + 2078]
                ic = np.concatenate([invcnt_for(256, 64 * s, 64), invcnt_for(8192, 2048 * s, 2048)], axis=1)
                m = {"zx": f32(np.concatenate([zc, zl], axis=1)), "invcnt": f32(ic)}
                m.update(hc)
                maps.append(m)
            r = _run(nc, maps)
            mixin = [{"mT": r[i]["mT"]} for i in range(8)]
        nc = _prog(('A', even), lambda: build_A(T128, even))
        maps = []
        for i, (b, s) in enumerate(cores):
            rows = np.zeros((2, 4, 2048), np.float32)
            for rr, mv in ((0, mod[l][2]), (1, mod[l][b])):
                rows[rr, 0] = mv[2]
                rows[rr, 1] = norm2_w[l]
                rows[rr, 2] = mv[4]
                rows[rr, 3] = mv[3]
            m = {"x": X[i], "w": w_out, "rows": rows}
            m.update(mixin[i])
            maps.append(m)
        r = _run(nc, maps)
        x1s = [r[i]["x1"] for i in range(8)]
        h2s = [r[i]["h2"] for i in range(8)]
        fin = (l == 3)
        nc = _prog(('peer', fin), lambda: build_peer(T128, final_norm=fin))
        keysT = np.ascontiguousarray(np.asarray(peer_keys[l], np.float32).reshape(16, 128, 128).transpose(2, 0, 1))
        qw, uu, vv_ = f32(peer_q_w[l]), f32(peer_u[l]), f32(peer_v[l])
        fnw = f32(np.asarray(final_norm_w).reshape(1, 2048))
        maps = []
        for i, (b, s) in enumerate(cores):
            g5 = np.stack([mod[l][2][5], mod[l][b][5]], 0).astype(np.float32)
            m = {"h2": h2s[i], "x1": x1s[i], "qw": qw, "keysT": keysT, "u": uu, "v": vv_, "g5": g5, "fnw": fnw}
            m.update(pc)
            maps.append(m)
        r = _run(nc, maps)
        X = [r[i]["x2"] for i in range(8)]
    out = np.zeros((2, 8192, 2048), np.float32)
    for i, (b, s) in enumerate(cores):
        out[b, 2048 * s:2048 * (s + 1)] = X[i][64:]
    return out
```
